# Optimizing a Trainium2 kernel written in Bass

```python
import numpy as np
import jax, jax.numpy as jnp
from jax import lax

D_MODEL = 1024
BATCH = 2
SEQ = 8192
DEPTH = 1

D_CONV = D_MODEL
CONV_WIDTH = 3
HEAD_DIM = 64
HEADS_PER_GROUP = 8
DILATED_GROUPS = ((128, 1), (512, 4), (2048, 16))
N_ATTN_HEADS = HEADS_PER_GROUP * len(DILATED_GROUPS)
D_ATTN = N_ATTN_HEADS * HEAD_DIM
D_ATTN_OUT = HEADS_PER_GROUP * HEAD_DIM
BLOCK = 128
ROT_DIM = HEAD_DIM // 4
ROPE_THETA = 500000.0
PEER_HEADS = 8
PEER_N_KEYS = 128
PEER_N_EXPERTS = PEER_N_KEYS * PEER_N_KEYS
PEER_TOPK = 16
PEER_D_HALF = 128
PEER_D_QUERY = 2 * PEER_D_HALF
PEER_CHUNK = 128
RMS_EPS = 1e-6

kernel_name = "hybrid_gatedconv_dilatedattn_peer"


def rms_norm(x, g):
    xf = x.astype(jnp.float32)
    y = xf * lax.rsqrt(jnp.mean(xf * xf, axis=-1, keepdims=True) + RMS_EPS)
    return (y * g.astype(jnp.float32)).astype(x.dtype)


def partial_rotary(t, positions):
    half = ROT_DIM // 2
    inv_freq = ROPE_THETA ** (-jnp.arange(half, dtype=jnp.float32) * (2.0 / ROT_DIM))
    ang = positions.astype(jnp.float32)[..., None] * inv_freq
    cos = jnp.cos(ang)[:, :, None, :]
    sin = jnp.sin(ang)[:, :, None, :]
    tr = t[..., :ROT_DIM].astype(jnp.float32)
    t1, t2 = tr[..., :half], tr[..., half:]
    rot = jnp.concatenate([t1 * cos - t2 * sin, t2 * cos + t1 * sin], axis=-1).astype(t.dtype)
    return jnp.concatenate([rot, t[..., ROT_DIM:]], axis=-1)


def dilated_window_attention(q, k, v, window, dilation):
    B, S, H, Dh = q.shape
    steps = window // dilation
    span = dilation * BLOCK
    sp = -(-S // span) * span
    nb = sp // span

    def blocks(t):
        t = jnp.pad(t, ((0, 0), (0, sp - S), (0, 0), (0, 0)))
        return t.reshape(B, nb, BLOCK, dilation, H, Dh)

    def with_prev(t):
        prev = jnp.pad(t, ((0, 0), (1, 0), (0, 0), (0, 0), (0, 0), (0, 0)))[:, :-1]
        return jnp.concatenate([prev, t], axis=2)

    qb = blocks(q)
    kk = with_prev(blocks(k))
    vv = with_prev(blocks(v))
    s = jnp.einsum('bnqrhd,bnkrhd->bnrhqk', qb, kk,
                   preferred_element_type=jnp.float32) * (Dh ** -0.5)
    qi = np.arange(BLOCK)[:, None]
    ki = np.arange(2 * BLOCK)[None, :]
    dist = BLOCK + qi - ki
    band = (dist >= 0) & (dist <= steps)
    valid = band[None] & ((np.arange(nb)[:, None, None] > 0) | (ki >= BLOCK)[None])
    s = jnp.where(valid[None, :, None, None], s, -jnp.inf)
    m = jnp.max(s, axis=-1, keepdims=True)
    p = jnp.exp(s - m)
    l = jnp.sum(p, axis=-1, keepdims=True)
    o = jnp.einsum('bnrhqk,bnkrhd->bnqrhd', (p / l).astype(v.dtype), vv)
    o = o.reshape(B, sp, H, Dh)[:, :S]
    lse = (m + jnp.log(l))[..., 0]
    lse = lse.transpose(0, 1, 4, 2, 3).reshape(B, sp, H)[:, :S]
    return o, lse


def hybrid_mixer(u, positions, w_in, conv_w, w_conv_out, w_attn_out, gate_bias, w_out):
    B, S, _ = u.shape
    sizes = [D_CONV, D_CONV, D_CONV, D_ATTN, D_ATTN, D_ATTN, D_MODEL, D_MODEL]
    split_points = [int(c) for c in np.cumsum(sizes)[:-1]]
    proj = u @ w_in
    b_gate, c_gate, xc, q, k, v, g_conv, g_attn = jnp.split(proj, split_points, axis=-1)

    z = c_gate * xc
    zp = jnp.pad(z, ((0, 0), (CONV_WIDTH - 1, 0), (0, 0)))
    conv = sum(conv_w[i] * zp[:, i:i + S] for i in range(CONV_WIDTH))
    y_conv = (b_gate * conv) @ w_conv_out

    q = partial_rotary(q.reshape(B, S, N_ATTN_HEADS, HEAD_DIM), positions)
    k = partial_rotary(k.reshape(B, S, N_ATTN_HEADS, HEAD_DIM), positions)
    v = v.reshape(B, S, N_ATTN_HEADS, HEAD_DIM)
    outs, lses = [], []
    for g, (window, dilation) in enumerate(DILATED_GROUPS):
        sl = slice(g * HEADS_PER_GROUP, (g + 1) * HEADS_PER_GROUP)
        o_g, lse_g = dilated_window_attention(q[:, :, sl], k[:, :, sl], v[:, :, sl], window, dilation)
        outs.append(o_g)
        lses.append(lse_g)
    wts = jax.nn.softmax(jnp.stack(lses, axis=0), axis=0)
    o = jnp.einsum('gbsh,gbshd->bshd', wts.astype(v.dtype), jnp.stack(outs, axis=0))
    y_attn = o.reshape(B, S, D_ATTN_OUT) @ w_attn_out

    merged = (jax.nn.sigmoid(g_conv + gate_bias[0]) * y_conv
              + jax.nn.sigmoid(g_attn + gate_bias[1]) * y_attn)
    return merged @ w_out


def peer_ffn(u, w_query, sub_keys, expert_down, expert_up):
    B, S, D = u.shape
    T = B * S
    xf = u.reshape(T, D)
    q = (xf @ w_query).reshape(T, PEER_HEADS, 2, PEER_D_HALF)
    s = jnp.einsum('thcd,hckd->thck', q, sub_keys, preferred_element_type=jnp.float32)
    top_s, top_i = lax.top_k(s, PEER_TOPK)
    cand_s = (top_s[:, :, 0, :, None] + top_s[:, :, 1, None, :]).reshape(T, PEER_HEADS, PEER_TOPK * PEER_TOPK)
    cand_i = (top_i[:, :, 0, :, None] * PEER_N_KEYS + top_i[:, :, 1, None, :]).reshape(T, PEER_HEADS, PEER_TOPK * PEER_TOPK)
    best_s, pos = lax.top_k(cand_s, PEER_TOPK)
    idx = jnp.take_along_axis(cand_i, pos, axis=-1)
    gates = jax.nn.softmax(best_s, axis=-1).astype(u.dtype)
    hk = PEER_HEADS * PEER_TOPK
    xr = xf.reshape(-1, PEER_CHUNK, D)
    ir = idx.reshape(-1, PEER_CHUNK, hk)
    gr = gates.reshape(-1, PEER_CHUNK, hk)

    def chunk(args):
        xc, ic, gc = args
        a = jax.nn.gelu(jnp.einsum('cd,ced->ce', xc, expert_down[ic]), approximate=False)
        return jnp.einsum('ce,ced->cd', gc * a, expert_up[ic])

    return lax.map(chunk, (xr, ir, gr)).reshape(B, S, D)


def setup_inputs(seed: int = 0) -> dict:
    key = jax.random.key(seed)
    ks = jax.random.split(key, 16)
    f32 = jnp.float32
    d_in = 3 * D_CONV + 3 * D_ATTN + 2 * D_MODEL
    x = jax.random.normal(ks[0], (BATCH, SEQ, D_MODEL), f32)
    offset = jax.random.randint(ks[1], (BATCH, 1), 0, 1024, dtype=jnp.int32)
    positions = (offset + jnp.arange(SEQ, dtype=jnp.int32)[None, :]).astype(jnp.int32)
    norm_mix = 1.0 + 0.01 * jax.random.normal(ks[2], (DEPTH, D_MODEL), f32)
    w_in = jax.random.normal(ks[3], (DEPTH, D_MODEL, d_in), f32) * D_MODEL ** -0.5
    conv_w = jax.random.normal(ks[4], (DEPTH, CONV_WIDTH, D_CONV), f32) * CONV_WIDTH ** -0.5
    w_conv_out = jax.random.normal(ks[5], (DEPTH, D_CONV, D_MODEL), f32) * D_CONV ** -0.5
    w_attn_out = jax.random.normal(ks[6], (DEPTH, D_ATTN_OUT, D_MODEL), f32) * D_ATTN_OUT ** -0.5
    gate_bias = 0.01 * jax.random.normal(ks[7], (DEPTH, 2, D_MODEL), f32)
    w_out = jax.random.normal(ks[8], (DEPTH, D_MODEL, D_MODEL), f32) * D_MODEL ** -0.5
    norm_ffn = 1.0 + 0.01 * jax.random.normal(ks[9], (DEPTH, D_MODEL), f32)
    peer_w_query = jax.random.normal(ks[10], (DEPTH, D_MODEL, PEER_HEADS * PEER_D_QUERY), f32) * D_MODEL ** -0.5
    peer_sub_keys = jax.random.normal(ks[11], (DEPTH, PEER_HEADS, 2, PEER_N_KEYS, PEER_D_HALF), f32) * PEER_D_HALF ** -0.5
    peer_down = jax.random.normal(ks[12], (DEPTH, PEER_N_EXPERTS, D_MODEL), f32) * D_MODEL ** -0.5
    peer_up = jax.random.normal(ks[13], (DEPTH, PEER_N_EXPERTS, D_MODEL), f32) * PEER_HEADS ** -0.5
    final_norm = 1.0 + 0.01 * jax.random.normal(ks[14], (D_MODEL,), f32)
    return {"x": x, "positions": positions, "norm_mix": norm_mix, "w_in": w_in,
            "conv_w": conv_w, "w_conv_out": w_conv_out, "w_attn_out": w_attn_out,
            "gate_bias": gate_bias, "w_out": w_out, "norm_ffn": norm_ffn,
            "peer_w_query": peer_w_query, "peer_sub_keys": peer_sub_keys,
            "peer_down": peer_down, "peer_up": peer_up, "final_norm": final_norm}


def reference(x, positions, norm_mix, w_in, conv_w, w_conv_out, w_attn_out, gate_bias, w_out,
              norm_ffn, peer_w_query, peer_sub_keys, peer_down, peer_up, final_norm):
    h = x
    for layer in range(DEPTH):
        h = h + hybrid_mixer(rms_norm(h, norm_mix[layer]), positions, w_in[layer], conv_w[layer],
                             w_conv_out[layer], w_attn_out[layer], gate_bias[layer], w_out[layer])
        h = h + peer_ffn(rms_norm(h, norm_ffn[layer]), peer_w_query[layer], peer_sub_keys[layer],
                         peer_down[layer], peer_up[layer])
    return rms_norm(h, final_norm)
```

```python
import numpy as np
import concourse.bass as bass
import concourse.mybir as mybir
from concourse.bass_utils import run_bass_kernel_spmd

F32 = mybir.dt.float32
BF16 = mybir.dt.bfloat16
I32 = mybir.dt.int32
U8 = mybir.dt.uint8
AF = mybir.ActivationFunctionType
ALU = mybir.AluOpType
AX = mybir.AxisListType

ENGS = ["pe", "act", "dve", "pool", "sp"]
NDMASEM = 8


class Sched:
    def __init__(self, nc):
        self.nc = nc
        self.ops = {e: [] for e in ENGS}
        self.last_w = {}
        self.readers = {}
        self.dma_count = {"sp": 0, "pool": 0, "act": 0}
        self.same_engine_sync = {"pe": False, "act": True, "dve": True, "pool": True, "sp": False}
        self.global_deps = []
        self.open_dmas = []

    def barrier(self):
        toks = []
        for e in ENGS:
            for i in range(len(self.ops[e]) - 1, -1, -1):
                if self.ops[e][i]["dma"] is None:
                    toks.append(("e", e, i))
                    break
        toks.extend(self.open_dmas)
        self.open_dmas = []
        self.global_deps = toks

    def _deps(self, reads, writes):
        deps = []
        for r in reads:
            t = self.last_w.get(r)
            if t is not None:
                deps.append(t)
        for w in writes:
            t = self.last_w.get(w)
            if t is not None:
                deps.append(t)
            deps.extend(self.readers.get(w, ()))
        return deps

    def _commit(self, tok, reads, writes):
        for r in reads:
            self.readers.setdefault(r, []).append(tok)
        for w in writes:
            self.last_w[w] = tok
            self.readers[w] = []

    @staticmethod
    def _excl(r, w):
        r2 = [k for k in r if not (isinstance(k, tuple) and k[0] == "ps")]
        w2 = list(w) + [k for k in r if isinstance(k, tuple) and k[0] == "ps"]
        return r2, w2

    def op(self, eng, fn, r=(), w=()):
        r, w = self._excl(r, w)
        deps = self._deps(r, w)
        idx = len(self.ops[eng])
        waits = []
        for t in list(deps) + self.global_deps:
            if t[0] == "e" and t[1] == eng and not self.same_engine_sync[eng]:
                continue
            waits.append(t)
        self.ops[eng].append(dict(fn=fn, waits=waits, dma=None))
        tok = ("e", eng, idx)
        self._commit(tok, r, w)
        return tok

    def dma(self, q, fn, r=(), w=()):
        r, w = self._excl(r, w)
        deps = self._deps(r, w)
        n = self.dma_count[q]
        self.dma_count[q] += 1
        slot, gen = n % NDMASEM, n // NDMASEM
        waits = list(deps) + [t for t in self.global_deps if not (t[0] == "e" and t[1] == q and not self.same_engine_sync[q])]
        if gen > 0:
            waits.append(("d", q, slot, gen))
        self.ops[q].append(dict(fn=fn, waits=waits, dma=(q, slot)))
        tok = ("d", q, slot, gen + 1)
        self.open_dmas.append(tok)
        self._commit(tok, r, w)
        return tok

    def emit(self, final_waits_engine="sp", final_toks=()):
        nc = self.nc
        needed = {e: set() for e in ENGS}
        for e in ENGS:
            for o in self.ops[e]:
                for t in o["waits"]:
                    if t[0] == "e":
                        needed[t[1]].add(t[2])
        for t in final_toks:
            if t[0] == "e":
                needed[t[1]].add(t[2])
        cnt = {}
        for e in ENGS:
            c = 0
            m = {}
            for i in range(len(self.ops[e])):
                if i in needed[e]:
                    c += 1
                    m[i] = c
            cnt[e] = m
        self.max_sem = {e: (max(cnt[e].values()) if cnt[e] else 0) for e in ENGS}
        esem = {e: nc.alloc_semaphore(name=f"prog_{e}") for e in ENGS}
        dsem = {q: [nc.alloc_semaphore(name=f"dma_{q}_{i}") for i in range(NDMASEM)]
                for q in ("sp", "pool", "act")}
        handles = {"pe": "tensor", "act": "scalar", "dve": "vector", "pool": "gpsimd", "sp": "sync"}

        def tokval(t):
            if t[0] == "e":
                return esem[t[1]], cnt[t[1]][t[2]]
            return dsem[t[1]][t[2]], 16 * t[3]

        def run_engine(ename, eng):
            waited = {}
            for i, o in enumerate(self.ops[ename]):
                for t in o["waits"]:
                    s, v = tokval(t)
                    key = id(s)
                    if waited.get(key, 0) >= v:
                        continue
                    eng.wait_ge(s, v)
                    waited[key] = v
                inst = o["fn"](eng)
                if o["dma"] is not None:
                    q, slot = o["dma"]
                    inst.then_inc(dsem[q][slot], 16)
                elif i in needed[ename]:
                    inst.then_inc(esem[ename], 1)
            if ename == final_waits_engine:
                for t in final_toks:
                    s, v = tokval(t)
                    eng.wait_ge(s, v)

        with nc.Block() as block:
            for ename in ENGS:
                deco = getattr(block, handles[ename])

                def body(eng, ename=ename):
                    run_engine(ename, eng)
                deco(body)


class Arena:
    def __init__(self, nc, nbytes, name="arena"):
        self.t = nc.alloc_sbuf_tensor(name, [128, nbytes], U8)
        self.n = nbytes
        self.top = 0

    def mark(self):
        return self.top

    def release(self, m):
        self.top = m

    def alloc(self, shape_free, dtype, parts=128):
        esz = {F32: 4, BF16: 2, I32: 4}[dtype]
        n = int(np.prod(shape_free)) * esz
        off = (self.top + 63) // 64 * 64
        assert off + n <= self.n, f"arena overflow {off}+{n}>{self.n}"
        self.top = off + n
        ap = self.t[0:parts, off:off + n].bitcast(dtype)
        if len(shape_free) > 1:
            names = " ".join(f"d{i}" for i in range(len(shape_free)))
            kw = {f"d{i}": int(s) for i, s in enumerate(shape_free)}
            ap = ap.rearrange(f"p ({names}) -> p {names}", **kw)
        return ap

    def view(self, off, shape_free, dtype, parts=128):
        esz = {F32: 4, BF16: 2, I32: 4}[dtype]
        n = int(np.prod(shape_free)) * esz
        assert off % 4 == 0 and off + n <= self.n, f"arena overflow {off}+{n}>{self.n}"
        ap = self.t[0:parts, off:off + n].bitcast(dtype)
        if len(shape_free) > 1:
            names = " ".join(f"d{i}" for i in range(len(shape_free)))
            kw = {f"d{i}": int(s) for i, s in enumerate(shape_free)}
            ap = ap.rearrange(f"p ({names}) -> p {names}", **kw)
        return ap


class Bump:
    def __init__(self, arena, lo, hi):
        self.a, self.lo, self.hi, self.top = arena, lo, hi, lo

    def alloc(self, shape_free, dtype, parts=128):
        esz = {F32: 4, BF16: 2, I32: 4}[dtype]
        n = int(np.prod(shape_free)) * esz
        off = (self.top + 63) // 64 * 64
        assert off + n <= self.hi, f"bump overflow {off}+{n}>{self.hi}"
        self.top = off + n
        return self.a.view(off, shape_free, dtype, parts)


KB = 1024
CUT = [0]
VBANK = [7]
D = 1024
T = 2048
NT = 16
C_B, C_C, C_X, C_Q, C_K, C_V, C_GC, C_GA = 0, 1024, 2048, 3072, 4608, 6144, 7680, 8704
GROUPS = [(1, 128), (4, 512), (16, 2048)]
MAGIC = 12582912.0
TWO_PI = 6.283185307179586
CW1 = 6.28125
CW2 = TWO_PI - CW1
PI_S = 3.1415925


def build(stage="full"):
    nc = bass.Bass("TRN2", target_bir_lowering=False)

    def din(name, shape, dt=F32):
        return nc.dram_tensor(name, list(shape), dt, kind="ExternalInput").ap()

    x_own = din("x_own", [T, D])
    x_halo = din("x_halo", [T, D])
    pos = din("pos", [1, 2 * T], I32)
    flag_d = din("halo_flag", [128, 1])
    w_in = din("w_in", [D, 9728])
    conv_w = din("conv_w", [3, D])
    w_co = din("w_conv_out", [D, D])
    w_ao = din("w_attn_out", [512, D])
    gbias = din("gate_bias", [2, D])
    w_out = din("w_out", [D, D])
    n_mix = din("norm_mix", [D])
    n_ffn = din("norm_ffn", [D])
    n_fin = din("final_norm", [D])
    wqry = din("peer_w_query", [D, 2048])
    skeys = din("peer_sub_keys", [16, 128, 128])
    if stage in ("full", "full_h2"):
        pdown = din("peer_down", [16384, D])
        pup = din("peer_up", [16384, D])
    c_ident = din("c_ident", [128, 128])
    c_mask = din("c_mask", [128, 256])
    c_e1 = din("c_e1", [128, 516])
    c_e2 = din("c_e2", [128, 2064])
    c_rope = din("c_rope", [128, 2])
    out = nc.dram_tensor("out", [T, D], F32, kind="ExternalOutput").ap()

    S = Sched(nc)
    A = Arena(nc, 206 * KB)
    ps = [nc.alloc_psum_tensor(f"psb{i}", [128, 512], F32) for i in range(8)]

    def psf(b, c0=0, n=512):
        return ps[b][:, c0:c0 + n]

    def psb(b):
        return ps[b][:, :].bitcast(BF16)

    w_in_v = w_in.rearrange("(c p) n -> p c n", p=128)
    w_co_v = w_co.rearrange("(c p) n -> p c n", p=128)
    w_ao_v = w_ao.rearrange("(c p) n -> p c n", p=128)
    w_out_v = w_out.rearrange("(c p) n -> p c n", p=128)
    wqry_v = wqry.rearrange("(c p) n -> p c n", p=128)

    CB = Bump(A, 0, 8 * KB)
    ident_f = CB.alloc([128], F32)
    ident_b = CB.alloc([128], BF16)
    mask_b = CB.alloc([256], BF16)
    e1_b = CB.alloc([516], BF16)
    e2_b = CB.alloc([2064], BF16)
    rope = CB.alloc([2], F32)
    flag = CB.alloc([1], F32)
    gmixT = CB.alloc([8, 1], F32)
    gffnT = CB.alloc([8, 1], F32)
    convwT = CB.alloc([3, 8, 1], F32)
    gbT = CB.alloc([2, 8, 1], F32)

    def dma(q, out_ap, in_ap, r=(), w=(), slow=False):
        def fn(e, out_ap=out_ap, in_ap=in_ap):
            if slow:
                return e.dma_start(out=out_ap, in_=in_ap, allow_slow_non_contiguous=True)
            return e.dma_start(out=out_ap, in_=in_ap)
        return S.dma(q, fn, r=r, w=w)

    WST = [A.view(190 * KB + i * 4096, [8, 128], F32) for i in range(4)]
    wst_n = [0]

    def cast_load(dst, src, ncols_total, dkey, src_is_w=True):
        kc = dst.shape[1]
        for c0 in range(0, ncols_total, 128):
            n = min(128, ncols_total - c0)
            i = wst_n[0] % 4
            wst_n[0] += 1
            st = WST[i][:, 0:kc, 0:n]
            dma("sp", st, src[:, :, c0:c0 + n], w=[("wst", i)])
            S.op("pool", lambda e, st=st, d=dst[:, :, c0:c0 + n]: e.tensor_copy(out=d, in_=st),
                 r=[("wst", i)], w=[dkey])

    def cast_load2(dst, src, dkey):
        n = dst.shape[1]
        for c0 in range(0, n, 1024):
            m = min(1024, n - c0)
            i = wst_n[0] % 4
            wst_n[0] += 1
            st = WST[i].rearrange("p a b -> p (a b)")[:, 0:m]
            dma("sp", st, src[:, c0:c0 + m], w=[("wst", i)])
            S.op("pool", lambda e, st=st, d=dst[:, c0:c0 + m]: e.tensor_copy(out=d, in_=st),
                 r=[("wst", i)], w=[dkey])

    dma("sp", ident_f, c_ident, w=["ident_f"])
    cast_load2(ident_b, c_ident, "ident_b")
    cast_load2(mask_b, c_mask, "mask_b")
    cast_load2(e1_b, c_e1, "e1_b")
    cast_load2(e2_b, c_e2, "e2_b")
    dma("sp", rope, c_rope, w=["rope"])
    dma("sp", flag, flag_d, w=["flag"])
    dma("sp", gmixT, n_mix.rearrange("(c p o) -> p c o", p=128, o=1), w=["gmixT"], slow=True)
    dma("sp", gffnT, n_ffn.rearrange("(c p o) -> p c o", p=128, o=1), w=["gffnT"], slow=True)
    for i_ in range(3):
        dma("sp", convwT[:, i_, :, :], conv_w[i_, :].rearrange("(c p o) -> p c o", p=128, o=1), w=["convwT"], slow=True)
    for j_ in range(2):
        dma("sp", gbT[:, j_, :, :], gbias[j_, :].rearrange("(c p o) -> p c o", p=128, o=1), w=["gbT"], slow=True)

    def early_exit(off):
        S.barrier()
        src = A.view(off, [NT, D], F32)
        fin = [dma("sp", out.rearrange("(t p) d -> p t d", p=128), src)]
        S.emit("sp", fin)
        return nc, S

    if stage == "const":
        return early_exit(0)

    def mm(out_ap, lhsT, rhs, start, stop, r=(), w=()):
        return S.op("pe", lambda e: e.matmul(out_ap, lhsT=lhsT, rhs=rhs, start=start, stop=stop), r=r, w=w)

    def tr(out_ap, in_ap, ident, r=(), w=()):
        return S.op("pe", lambda e: e.transpose(out_ap, in_ap, ident), r=r, w=w)

    def act(out_ap, in_ap, func, r=(), w=(), **kw):
        return S.op("act", lambda e: e.activation(out=out_ap, in_=in_ap, func=func, **kw), r=r, w=w)

    def tt(eng, out_ap, in0, in1, op, r=(), w=()):
        return S.op(eng, lambda e: e.tensor_tensor(out=out_ap, in0=in0, in1=in1, op=op), r=r, w=w)

    def ts(eng, out_ap, in0, s1, s2, op0, op1=None, r=(), w=()):
        if op1 is None:
            return S.op(eng, lambda e: e.tensor_scalar(out=out_ap, in0=in0, scalar1=s1, scalar2=None, op0=op0), r=r, w=w)
        return S.op(eng, lambda e: e.tensor_scalar(out=out_ap, in0=in0, scalar1=s1, scalar2=s2, op0=op0, op1=op1), r=r, w=w)

    def stt(out_ap, in0, scalar, in1, op0, op1, r=(), w=()):
        return S.op("dve", lambda e: e.scalar_tensor_tensor(out=out_ap, in0=in0, scalar=scalar, in1=in1, op0=op0, op1=op1), r=r, w=w)

    def cp(eng, out_ap, in_ap, r=(), w=()):
        return S.op(eng, lambda e: e.tensor_copy(out=out_ap, in_=in_ap), r=r, w=w)

    def xkeys(t0, n, step=1):
        return [("xT", i) for i in range(t0 // 128, (t0 + (n - 1) * step) // 128 + 1)]

    def load_w(dst, src_v, c0, ncols, key):
        return cast_load(dst, src_v[:, :, c0:c0 + ncols], ncols, key)

    def proj_fm(ps_ap, pskey, wt, wkey, xT, t0, n, kc=8):
        for c in range(kc):
            mm(ps_ap, wt[:, c, :], xT[:, c, t0:t0 + n], c == 0, c == kc - 1,
               r=[wkey] + xkeys(t0, n), w=[pskey])

    xT_all = A.view(8 * KB, [8, 2 * T], BF16)
    cosT = A.view(72 * KB, [2 * T], F32)
    sinT = A.view(88 * KB, [2 * T], F32)
    oT = A.view(104 * KB, [4, T], BF16)

    TB = Bump(A, 104 * KB, 190 * KB)
    posi = TB.alloc([2 * T], I32)
    ang = TB.alloc([2 * T], F32)
    uu = TB.alloc([2 * T], F32)
    kf = TB.alloc([2 * T], F32)
    rr = TB.alloc([2 * T], F32)
    dma("sp", posi, pos[0, :].partition_broadcast(128), w=["posi"])
    cp("dve", ang, posi, r=["posi"], w=["ang"])
    ts("dve", ang, ang, rope[:, 0:1], None, ALU.mult, r=["ang", "rope"], w=["ang"])

    def sin_table(dst, dkey, shift, scale):
        ts("dve", uu, ang, float(shift), None, ALU.add, r=["ang"], w=["uu"])
        ts("dve", kf, uu, 1.0 / TWO_PI, MAGIC, ALU.mult, ALU.add, r=["uu"], w=["kf"])
        ts("dve", kf, kf, -MAGIC, None, ALU.add, r=["kf"], w=["kf"])
        stt(rr, kf, -CW1, uu, ALU.mult, ALU.add, r=["kf", "uu"], w=["rr"])
        stt(rr, kf, -CW2, rr, ALU.mult, ALU.add, r=["kf", "rr"], w=["rr"])
        ts("dve", rr, rr, -PI_S, PI_S, ALU.max, ALU.min, r=["rr"], w=["rr"])
        act(dst, rr, AF.Sin, r=["rr", "rope"], w=[dkey], scale=scale)

    sin_table(cosT, "cosT", np.pi / 2, 1.0)
    sin_table(sinT, "sinT", 0.0, rope[:, 1:2])
    S.barrier()
    if stage == "rope":
        return early_exit(72 * KB)

    def norm_phase(src_tiles, gT, dstT, dkey, B, tile0=0):
        xin = [B.alloc([D], F32) for _ in range(2)]
        xs = [B.alloc([D], BF16) for _ in range(2)]
        junk = B.alloc([D], BF16)
        ss = [B.alloc([1], F32) for _ in range(2)]
        sd = [B.alloc([1], F32) for _ in range(2)]
        rstd = [B.alloc([1], F32) for _ in range(2)]
        for i, src in enumerate(src_tiles):
            k = i % 2
            if src[0] == "dram":
                dma("sp", xin[k], src[1], w=[("xin", k)])
                xi, xkey = xin[k], ("xin", k)
            else:
                xi, xkey = src[1], src[2]
            if CUT[0] == 9:
                continue
            act(junk, xi, AF.Square, r=[xkey], w=["junk", ("ss", k)], accum_out=ss[k])
            if CUT[0] == 1:
                continue
            act(sd[k], ss[k], AF.Sqrt, r=[("ss", k)], w=[("sd", k)], scale=1.0 / D, bias=1e-6)
            if CUT[0] == 2:
                continue
            S.op("dve", lambda e, k=k: e.reciprocal(out=rstd[k], in_=sd[k]), r=[("sd", k)], w=[("rstd", k)])
            if CUT[0] == 3:
                continue
            act(xs[k], xi, AF.Copy, r=[xkey, ("rstd", k)], w=[("xs", k)], scale=rstd[k])
            if CUT[0] == 4:
                continue
            pb = psb(k)
            for c in range(8):
                tr(pb[:, c * 128:(c + 1) * 128], xs[k][:, c * 128:(c + 1) * 128], ident_b,
                   r=[("xs", k), "ident_b"], w=[("ps", k)])
            if CUT[0] == 5:
                continue
            tt("dve", dstT[:, :, (tile0 + i) * 128:(tile0 + i + 1) * 128],
               pb.rearrange("p (c t) -> p c t", c=8), gT[:, :, 0:1].to_broadcast([128, 8, 128]), ALU.mult,
               r=[("ps", k)], w=[(dkey, tile0 + i)])

    PB = Bump(A, 120 * KB, 190 * KB)
    tiles = [("dram", x_halo[i * 128:(i + 1) * 128, :]) for i in range(NT)] + \
            [("dram", x_own[i * 128:(i + 1) * 128, :]) for i in range(NT)]
    if stage.startswith("A") and len(stage) > 1:
        tiles = tiles[:int(stage[1:])]
    norm_phase(tiles, gmixT, xT_all, "xT", PB)
    S.barrier()
    if stage.startswith("A"):
        return early_exit(8 * KB)

    AB = Bump(A, 120 * KB, 190 * KB)
    wq_t = [AB.alloc([8, 128], BF16) for _ in range(2)]
    wqs_t = [AB.alloc([8, 128], BF16) for _ in range(2)]
    wk_t = [AB.alloc([8, 128], BF16) for _ in range(2)]
    wks_t = [AB.alloc([8, 128], BF16) for _ in range(2)]
    wv_t = [AB.alloc([8, 128], BF16) for _ in range(2)]
    Qbuf = AB.alloc([T], BF16)
    Kbuf = AB.alloc([2 * T], BF16)
    Vbuf = AB.alloc([32, 2, 66], BF16)
    U = [AB.alloc([16, 130], BF16) for _ in range(3)]
    tA = [AB.alloc([512], F32) for _ in range(2)]
    tB = [AB.alloc([512], F32) for _ in range(2)]
    Pt = [AB.alloc([256], BF16) for _ in range(4)]
    otok = [AB.alloc([2, 64], BF16) for _ in range(2)]
    rl = [AB.alloc([2, 1], F32) for _ in range(2)]
    for k in range(2):
        S.op("pool", lambda e, k=k: e.memset(wqs_t[k], 0.0), w=[("wqs", k)])
        S.op("pool", lambda e, k=k: e.memset(wks_t[k], 0.0), w=[("wks", k)])

    def build_sw(dst, dkey, src, skey):
        for hh in range(2):
            b = hh * 64
            cp("pool", dst[:, :, b:b + 8], src[:, :, b + 8:b + 16], r=[skey], w=[dkey])
            cp("pool", dst[:, :, b + 8:b + 16], src[:, :, b:b + 8], r=[skey], w=[dkey])

    def perm_out(buf, nblk, dil, span, lt0, cs):
        v4 = buf[:, 0:nblk * span].rearrange("p (n r i) -> p n i r", n=nblk, r=dil, i=128)
        if cs >= span:
            n0, nn = lt0 // span, cs // span
            return v4[:, n0:n0 + nn, :, :], (lambda a: a[:, 0:cs].rearrange("p (n i r) -> p n i r", n=nn, i=128, r=dil))
        n0 = lt0 // span
        i0, ni = (lt0 % span) // dil, cs // dil
        return v4[:, n0, i0:i0 + ni, :], (lambda a: a[:, 0:cs].rearrange("p (i r) -> p i r", r=dil))

    wcnt = [0]
    pcnt = [0]
    for pp in range(4):
        for g, (dil, span) in enumerate(GROUPS):
            hp = g * 4 + pp
            nb = T // span
            k0 = T - span
            nk = span + T
            wi = wcnt[0] % 2
            wcnt[0] += 1
            load_w(wq_t[wi], w_in_v, C_Q + hp * 128, 128, ("wq", wi))
            build_sw(wqs_t[wi], ("wqs", wi), wq_t[wi], ("wq", wi))
            load_w(wk_t[wi], w_in_v, C_K + hp * 128, 128, ("wk", wi))
            build_sw(wks_t[wi], ("wks", wi), wk_t[wi], ("wk", wi))
            load_w(wv_t[wi], w_in_v, C_V + hp * 128, 128, ("wv", wi))

            def rot_chunk(wt, wkey, wst, wskey, t0, cs, buf, bkey, nblk, lt0):
                i = pcnt[0] % 2
                pcnt[0] += 1
                b0 = i * 2
                proj_fm(psf(b0, 0, cs), ("ps", b0), wt, wkey, xT_all, t0, cs)
                proj_fm(psf(b0 + 1, 0, cs), ("ps", b0 + 1), wst, wskey, xT_all, t0, cs)
                tt("dve", tA[i][:, 0:cs], psf(b0, 0, cs), cosT[:, t0:t0 + cs], ALU.mult,
                   r=[("ps", b0), "cosT"], w=[("tA", i)])
                tt("dve", tB[i][:, 0:cs], psf(b0 + 1, 0, cs), sinT[:, t0:t0 + cs], ALU.mult,
                   r=[("ps", b0 + 1), "sinT"], w=[("tB", i)])
                ov, inr = perm_out(buf, nblk, dil, span, lt0, cs)
                tt("dve", ov, inr(tA[i]), inr(tB[i]), ALU.add, r=[("tA", i), ("tB", i)], w=[bkey])

            for j in range(4):
                rot_chunk(wq_t[wi], ("wq", wi), wqs_t[wi], ("wqs", wi), T + j * 512, 512, Qbuf, "Q", nb, j * 512)
            if stage == "att1":
                return early_exit(120 * KB)
            lt = 0
            while lt < nk:
                cs = min(512, nk - lt)
                rot_chunk(wk_t[wi], ("wk", wi), wks_t[wi], ("wks", wi), k0 + lt, cs, Kbuf, "K", nb + 1, lt)
                lt += cs
            if stage == "att2":
                return early_exit(120 * KB)
            nvb = (nb + 1) * dil
            S.op("dve", lambda e, nvb=nvb: e.memset(Vbuf[:, 0:nvb, :, 64:65], 1.0), w=["V"])
            ts("dve", Vbuf[:, 0:dil, :, 64:65], Vbuf[:, 0:dil, :, 64:65], flag[:, 0:1], None, ALU.mult,
               r=["flag"], w=["V"])
            if stage == "att3a":
                return early_exit(120 * KB)
            for blk in range(nvb):
                n_, r_ = blk // dil, blk % dil
                st = k0 + n_ * span + r_
                s4 = 6 + blk % 2
                pv = psf(s4, 0, 128)
                for c in range(8):
                    mm(pv, xT_all[:, c, st:st + 127 * dil + 1:dil], wv_t[wi][:, c, :], c == 0, c == 7,
                       r=[("wv", wi)] + xkeys(st, 128, dil), w=[("ps", s4)])
                if stage == "att3b":
                    continue
                for hh_ in range(2):
                    cp("dve", Vbuf[:, blk, hh_, 0:64], pv[:, hh_ * 64:(hh_ + 1) * 64],
                       r=[("ps", s4)], w=["V"])
            if stage in ("att3", "att3b"):
                return early_exit(120 * KB)
            Kb = Kbuf[:, 0:nk].rearrange("p (n r i) -> p n r i", n=nb + 1, r=dil, i=128)
            Qb = Qbuf[:, 0:T].rearrange("p (n r i) -> p n r i", n=nb, r=dil, i=128)
            Ug = U[g].rearrange("p b (h d) -> p b h d", h=2)
            items = [(b16, hh) for b16 in range(16) for hh in range(2)]

            def st_scores(ii, b16, hh):
                n_, r_ = b16 // dil, b16 % dil
                rows = slice(hh * 64, hh * 64 + 64)
                s2 = 4 + ii % 2
                pS = psf(s2, 0, 256)
                mm(pS[:, 0:128], Kb[rows, n_, r_, :], Qb[rows, n_, r_, :], True, True, r=["K", "Q"], w=[("ps", s2)])
                mm(pS[:, 128:256], Kb[rows, n_ + 1, r_, :], Qb[rows, n_, r_, :], True, True, r=["K", "Q"], w=[("ps", s2)])
                p4 = ii % 4
                act(Pt[p4], pS, AF.Exp, r=[("ps", s2)], w=[("Pt", p4)], scale=0.125)
                tt("dve", Pt[p4], Pt[p4], mask_b, ALU.mult, r=[("Pt", p4), "mask_b"], w=[("Pt", p4)])

            def st_pv(ii, b16, hh):
                n_, r_ = b16 // dil, b16 % dil
                p4 = ii % 4
                b6 = 6 + ii % 2
                pO = psf(b6, 0, 65)
                mm(pO, Pt[p4][:, 0:128], Vbuf[:, n_ * dil + r_, hh, 0:65], True, False, r=[("Pt", p4), "V"], w=[("ps", b6)])
                mm(pO, Pt[p4][:, 128:256], Vbuf[:, (n_ + 1) * dil + r_, hh, 0:65], False, True, r=[("Pt", p4), "V"], w=[("ps", b6)])
                cp("dve", Ug[:, b16, hh, :], pO, r=[("ps", b6)], w=[("U", g)])

            for ii in range(len(items) + 1):
                if ii < len(items):
                    st_scores(ii, *items[ii])
                if ii >= 1:
                    st_pv(ii - 1, *items[ii - 1])
            if stage == "att4":
                return early_exit(120 * KB)
        for tau in range(NT):
            c2 = tau % 2
            pC = psf(c2, 0, 130)
            ops = [(ident_b, U[0][:, tau, :], ("U", 0))]
            for r_ in range(4):
                off = 4 - r_ + (tau % 4) * 128
                ops.append((e1_b[:, off:off + 128], U[1][:, (tau // 4) * 4 + r_, :], ("U", 1)))
            for r_ in range(16):
                off = 16 - r_ + tau * 128
                ops.append((e2_b[:, off:off + 128], U[2][:, r_, :], ("U", 2)))
            for oi, (l_, r_ap, uk) in enumerate(ops):
                mm(pC, l_, r_ap, oi == 0, oi == len(ops) - 1, r=[uk, "ident_b", "e1_b", "e2_b"], w=[("ps", c2)])
            pC3 = pC.rearrange("p (h d) -> p h d", h=2)
            S.op("dve", lambda e, c2=c2, pC3=pC3: e.reciprocal(out=rl[c2], in_=pC3[:, :, 64:65]),
                 r=[("ps", c2)], w=[("rl", c2)])
            tt("dve", otok[c2], pC3[:, :, 0:64], rl[c2][:, :, 0:1].to_broadcast([128, 2, 64]), ALU.mult,
               r=[("ps", c2), ("rl", c2)], w=[("otok", c2)])
            pT = psb(2 + c2)[:, 0:128]
            tr(pT, otok[c2].rearrange("p h d -> p (h d)"), ident_b, r=[("otok", c2), "ident_b"], w=[("ps", 2 + c2)])
            act(oT[:, pp, tau * 128:(tau + 1) * 128], pT, AF.Copy, r=[("ps", 2 + c2)], w=["oT"])
    S.barrier()

    uT = A.view(120 * KB, [8, T], BF16)
    VB = Bump(A, 152 * KB, 190 * KB)
    zbuf = VB.alloc([T + 2], F32)
    acc = VB.alloc([T], F32)
    ctmp = [VB.alloc([512], F32) for _ in range(2)]
    wb_t = [VB.alloc([8, 128], BF16) for _ in range(2)]
    wc_t = [VB.alloc([8, 128], BF16) for _ in range(2)]
    wx_t = [VB.alloc([8, 128], BF16) for _ in range(2)]
    pc = [0]
    for ft in range(8):
        wi = ft % 2
        load_w(wc_t[wi], w_in_v, C_C + ft * 128, 128, ("wc", wi))
        load_w(wx_t[wi], w_in_v, C_X + ft * 128, 128, ("wx", wi))
        load_w(wb_t[wi], w_in_v, C_B + ft * 128, 128, ("wb", wi))
        chunks = [(T - 2, 2, 0)] + [(T + j * 512, 512, 2 + j * 512) for j in range(4)]
        for (t0, cs, z0) in chunks:
            i = pc[0] % 2
            pc[0] += 1
            b0 = i * 2
            proj_fm(psf(b0, 0, cs), ("ps", b0), wc_t[wi], ("wc", wi), xT_all, t0, cs)
            proj_fm(psf(b0 + 1, 0, cs), ("ps", b0 + 1), wx_t[wi], ("wx", wi), xT_all, t0, cs)
            act(ctmp[i][:, 0:cs], psf(b0, 0, cs), AF.Copy, r=[("ps", b0)], w=[("ctmp", i)])
            tt("dve", zbuf[:, z0:z0 + cs], ctmp[i][:, 0:cs], psf(b0 + 1, 0, cs), ALU.mult,
               r=[("ctmp", i), ("ps", b0 + 1)], w=["z"])
        ts("dve", acc, zbuf[:, 0:T], convwT[:, 0, ft, :], None, ALU.mult, r=["z", "convwT"], w=["acc"])
        stt(acc, zbuf[:, 1:T + 1], convwT[:, 1, ft, :], acc, ALU.mult, ALU.add, r=["z", "acc"], w=["acc"])
        stt(acc, zbuf[:, 2:T + 2], convwT[:, 2, ft, :], acc, ALU.mult, ALU.add, r=["z", "acc"], w=["acc"])
        for j in range(4):
            i = pc[0] % 2
            pc[0] += 1
            b0 = i * 2
            proj_fm(psf(b0), ("ps", b0), wb_t[wi], ("wb", wi), xT_all, T + j * 512, 512)
            tt("dve", uT[:, ft, j * 512:(j + 1) * 512], acc[:, j * 512:(j + 1) * 512], psf(b0), ALU.mult,
               r=["acc", ("ps", b0)], w=["uT"])
    S.barrier()

    mergedT = A.view(72 * KB, [8, T], BF16)
    MB = Bump(A, 152 * KB, 190 * KB)
    wco_t = [MB.alloc([8, 128], BF16) for _ in range(2)]
    wgc_t = [MB.alloc([8, 128], BF16) for _ in range(2)]
    wao_t = [MB.alloc([4, 128], BF16) for _ in range(2)]
    wga_t = [MB.alloc([8, 128], BF16) for _ in range(2)]
    sg = [MB.alloc([512], F32) for _ in range(2)]
    sga = [MB.alloc([512], F32) for _ in range(2)]
    m1 = [MB.alloc([512], F32) for _ in range(2)]
    m2 = [MB.alloc([512], F32) for _ in range(2)]
    for mt in range(8):
        wi = mt % 2
        load_w(wco_t[wi], w_co_v, mt * 128, 128, ("wco", wi))
        load_w(wgc_t[wi], w_in_v, C_GC + mt * 128, 128, ("wgc", wi))
        load_w(wao_t[wi], w_ao_v, mt * 128, 128, ("wao", wi))
        load_w(wga_t[wi], w_in_v, C_GA + mt * 128, 128, ("wga", wi))
        for j in range(4):
            i = j % 2
            b = i * 4
            to = j * 512
            for ft in range(8):
                mm(psf(b), wco_t[wi][:, ft, :], uT[:, ft, to:to + 512], ft == 0, ft == 7, r=[("wco", wi), "uT"], w=[("ps", b)])
            proj_fm(psf(b + 1), ("ps", b + 1), wgc_t[wi], ("wgc", wi), xT_all, T + to, 512)
            for p_ in range(4):
                mm(psf(b + 2), wao_t[wi][:, p_, :], oT[:, p_, to:to + 512], p_ == 0, p_ == 3, r=[("wao", wi), "oT"], w=[("ps", b + 2)])
            proj_fm(psf(b + 3), ("ps", b + 3), wga_t[wi], ("wga", wi), xT_all, T + to, 512)
            act(sg[i], psf(b + 1), AF.Sigmoid, r=[("ps", b + 1), "gbT"], w=[("sg", i)], bias=gbT[:, 0, mt, :])
            act(sga[i], psf(b + 3), AF.Sigmoid, r=[("ps", b + 3), "gbT"], w=[("sga", i)], bias=gbT[:, 1, mt, :])
            tt("dve", m1[i], sg[i], psf(b), ALU.mult, r=[("sg", i), ("ps", b)], w=[("m1", i)])
            tt("dve", m2[i], sga[i], psf(b + 2), ALU.mult, r=[("sga", i), ("ps", b + 2)], w=[("m2", i)])
            tt("pool", mergedT[:, mt, to:to + 512], m1[i], m2[i], ALU.add, r=[("m1", i), ("m2", i)], w=["mergedT"])
    S.barrier()

    h = A.view(8 * KB, [NT, D], F32)
    wout = A.view(104 * KB, [8, D], BF16)
    load_w(wout[:, :, 0:512], w_out_v, 0, 512, "wout")
    load_w(wout[:, :, 512:1024], w_out_v, 512, 512, "wout")
    for tau in range(NT):
        dma("sp", h[:, tau, :], x_own[tau * 128:(tau + 1) * 128, :], w=[("h", tau)])
        for half in range(2):
            b = (2 * tau + half) % 4
            for c in range(8):
                mm(psf(b), mergedT[:, c, tau * 128:(tau + 1) * 128], wout[:, c, half * 512:(half + 1) * 512],
                   c == 0, c == 7, r=["mergedT", "wout"], w=[("ps", b)])
            tt("dve", h[:, tau, half * 512:(half + 1) * 512], psf(b), h[:, tau, half * 512:(half + 1) * 512], ALU.add,
               r=[("ps", b), ("h", tau)], w=[("h", tau)])
    S.barrier()

    finals = []
    if stage == "mixer":
        for tau in range(NT):
            finals.append(dma("sp", out[tau * 128:(tau + 1) * 128, :], h[:, tau, :], r=[("h", tau)]))
        S.emit("sp", finals)
        return nc, S

    build_peer(nc, S, A, ps, psf, psb, h, locals())
    return nc, S


def build_peer(nc, S, A, ps, psf, psb, h, L):
    mm, tr, act, tt, ts, stt, cp, dma = (L[n] for n in ("mm", "tr", "act", "tt", "ts", "stt", "cp", "dma"))
    cast_load, proj_fm, norm_phase = L["cast_load"], L["proj_fm"], L["norm_phase"]
    WST, wst_n = L["WST"], L["wst_n"]
    ident_f, ident_b, gffnT = L["ident_f"], L["ident_b"], L["gffnT"]
    wqry_v, skeys, pdown, pup, n_fin, out, stage = (L[n] for n in ("wqry_v", "skeys", "pdown", "pup", "n_fin", "out", "stage"))
    NEG = -1.0e30
    NBLK = 128

    dnT_d = nc.dram_tensor("dnT_scr", [NBLK, 128, 8, 128], BF16, kind="Internal").ap()
    up_d = nc.dram_tensor("up_scr", [NBLK * 128, D], BF16, kind="Internal").ap()
    wq_d = nc.dram_tensor("wq_scr", [16, 128, 8, 128], BF16, kind="Internal").ap()

    hnT = A.view(72 * KB, [8, T], BF16)
    PBm = Bump(A, 104 * KB, 190 * KB)
    skT = PBm.alloc([16, 128], BF16)
    lo = PBm.top

    NB_ = Bump(A, lo, 190 * KB)
    norm_phase([("sbuf", h[:, i, :], ("h", i)) for i in range(NT)], gffnT, hnT, "hnT", NB_)
    SB_ = Bump(A, NB_.top, 190 * KB)
    sk_b = [SB_.alloc([128], BF16) for _ in range(2)]
    for j in range(16):
        i = wst_n[0] % 4
        wst_n[0] += 1
        st = WST[i][:, 0, :]
        dma("sp", st, skeys[j, :, :], w=[("wst", i)])
        cp("pool", sk_b[j % 2], st, r=[("wst", i)], w=[("sk_b", j % 2)])
        b = 4 + j % 2
        tr(psb(b)[:, 0:128], sk_b[j % 2], ident_b, r=[("sk_b", j % 2), "ident_b"], w=[("ps", b)])
        act(skT[:, j, :], psb(b)[:, 0:128], AF.Copy, r=[("ps", b)], w=["skT"])
    S.barrier()

    XB = Bump(A, lo, 190 * KB)
    Dn_b = [XB.alloc([D], BF16) for _ in range(2)]
    Up_b = [XB.alloc([D], BF16) for _ in range(2)]
    DnT_o = [XB.alloc([8, 128], BF16) for _ in range(2)]
    stg = {}

    def pre_load(blk):
        i = wst_n[0] % 4
        wst_n[0] += 1
        i2 = wst_n[0] % 4
        wst_n[0] += 1
        dma("sp", WST[i].rearrange("p a b -> p (a b)"), pdown[blk * 128:(blk + 1) * 128, :], w=[("wst", i)])
        dma("sp", WST[i2].rearrange("p a b -> p (a b)"), pup[blk * 128:(blk + 1) * 128, :], w=[("wst", i2)])
        stg[blk] = (i, i2)

    wq_o = [XB.alloc([8, 128], BF16) for _ in range(2)]
    for j in range(16):
        i = wst_n[0] % 4
        wst_n[0] += 1
        dma("sp", WST[i], wqry_v[:, :, j * 128:(j + 1) * 128], w=[("wst", i)])
        cp("dve", wq_o[j % 2], WST[i], r=[("wst", i)], w=[("wq_o", j % 2)])
        dma("sp", wq_d[j], wq_o[j % 2], r=[("wq_o", j % 2)], w=[("wq_d", j)])
    pre_load(0)
    for blk in range(NBLK):
        if blk + 1 < NBLK:
            pre_load(blk + 1)
        i, i2 = stg.pop(blk)
        k2 = blk % 2
        act(Dn_b[k2], WST[i].rearrange("p a b -> p (a b)"), AF.Copy, r=[("wst", i)], w=[("Dn_b", k2)])
        cp("dve", Up_b[k2], WST[i2].rearrange("p a b -> p (a b)"), r=[("wst", i2)], w=[("Up_b", k2)])
        b = 4 + blk % 4
        for c in range(8):
            tr(psb(b)[:, c * 128:(c + 1) * 128], Dn_b[k2][:, c * 128:(c + 1) * 128], ident_b,
               r=[("Dn_b", k2), "ident_b"], w=[("ps", b)])
        cp("dve", DnT_o[k2], psb(b).rearrange("p (c e) -> p c e", c=8), r=[("ps", b)], w=[("DnT_o", k2)])
        dma("sp", dnT_d[blk], DnT_o[k2], r=[("DnT_o", k2)], w=[("dnT_d", blk)])
        dma("sp", up_d[blk * 128:(blk + 1) * 128, :], Up_b[k2], r=[("Up_b", k2)], w=[("up_d", blk)])
    S.barrier()

    GB = Bump(A, lo, 190 * KB)
    s_sb = GB.alloc([2, 16, 128], F32)
    E2 = GB.alloc([2, 8, 128], F32)
    E1q = GB.alloc([2, 8, 128], F32)
    Dg = GB.alloc([2, 8, 128], BF16)
    lo2 = GB.top
    s5 = s_sb.rearrange("p t (h c) k -> p t h c k", c=2)

    for G in range(NT // 2):
        t0 = G * 256
        SU = Bump(A, lo2, 190 * KB)
        qpT = SU.alloc([16, 256], BF16)
        wq_t = [SU.alloc([8, 128], BF16) for _ in range(2)]
        TT = []
        for _t in range(2):
            TT.append(dict(cand=SU.alloc([8, 256], F32), vals=SU.alloc([256], F32),
                           work=[SU.alloc([256], F32) for _ in range(2)], best=SU.alloc([8, 16], F32),
                           m3=SU.alloc([8, 8], F32), thr=SU.alloc([8, 1], F32), dd=SU.alloc([8, 16], F32),
                           Z=SU.alloc([8, 1], F32), rZ=SU.alloc([8, 1], F32), gthr=SU.alloc([8, 1], F32)))
        for j in range(16):
            wi = j % 2
            dma("sp", wq_t[wi], wq_d[j], r=[("wq_d", j)], w=[("wqp", wi)])
            b = 4 + j % 2
            proj_fm(psf(b, 0, 256), ("ps", b), wq_t[wi], ("wqp", wi), hnT, t0, 256)
            act(qpT[:, j, :], psf(b, 0, 256), AF.Copy, r=[("ps", b)], w=["qpT"])
        def tile_gen(tt_):
            cand, vals, work, best, m3, thr, dd, Z, rZ, gthr = (TT[tt_][n] for n in
                ("cand", "vals", "work", "best", "m3", "thr", "dd", "Z", "rZ", "gthr"))
            K_ = lambda n: (n, tt_)
            for jj in range(4):
                b = 6 + jj % 2
                for j4 in range(4):
                    j = jj * 4 + j4
                    mm(psf(b, j4 * 128, 128), qpT[:, j, tt_ * 128:(tt_ + 1) * 128], skT[:, j, :], True, True,
                       r=["qpT", "skT"], w=[("ps", b)])
                cp("dve", s_sb[:, tt_, jj * 4:(jj + 1) * 4, :], psf(b).rearrange("p (j k) -> p j k", j=4),
                   r=[("ps", b)], w=[("s_sb", tt_, jj)])
                yield
            skeys_ = [("s_sb", tt_, jj) for jj in range(4)]
            v_a = vals.rearrange("p (h c a o) -> p h c a o", h=8, c=2, a=16, o=1)
            v_b = vals.rearrange("p (h c o a) -> p h c o a", h=8, c=2, a=16, o=1)
            v3 = vals.rearrange("p (j a) -> p j a", j=16)
            for j in range(16):
                src = s_sb[:, tt_, j, :]
                sk = [("s_sb", tt_, j // 4)]
                S.op("dve", lambda e, j=j, src=src: e.max(out=v3[:, j, 0:8], in_=src), r=sk, w=[K_("vals")])
                yield
                S.op("dve", lambda e, j=j, src=src: e.match_replace(out=work[0][:, 0:128], in_to_replace=v3[:, j, 0:8],
                                                                    in_values=src, imm_value=NEG),
                     r=sk + [K_("vals")], w=[K_("work0")])
                yield
                S.op("dve", lambda e, j=j: e.max(out=v3[:, j, 8:16], in_=work[0][:, 0:128]), r=[K_("work0")], w=[K_("vals")])
                yield
            for hd in range(8):
                tt("dve", cand[:, hd, :].rearrange("p (a b) -> p a b", a=16),
                   v_a[:, hd, 0, :, 0:1].to_broadcast([128, 16, 16]),
                   v_b[:, hd, 1, 0:1, :].to_broadcast([128, 16, 16]), ALU.add, r=[K_("vals")], w=[K_("cand")])
                yield
            for hd in range(8):
                ch = cand[:, hd, :]
                S.op("dve", lambda e, hd=hd, ch=ch: e.max(out=best[:, hd, 0:8], in_=ch), r=[K_("cand")], w=[K_("best")])
                yield
                S.op("dve", lambda e, hd=hd, ch=ch: e.match_replace(out=work[0], in_to_replace=best[:, hd, 0:8],
                                                                    in_values=ch, imm_value=NEG),
                     r=[K_("cand"), K_("best")], w=[K_("work0")])
                yield
                S.op("dve", lambda e, hd=hd: e.max(out=best[:, hd, 8:16], in_=work[0]), r=[K_("work0")], w=[K_("best")])
                yield
                S.op("dve", lambda e, hd=hd: e.match_replace(out=work[1], in_to_replace=best[:, hd, 8:16],
                                                             in_values=work[0], imm_value=NEG),
                     r=[K_("work0"), K_("best")], w=[K_("work1")])
                yield
                S.op("dve", lambda e, hd=hd: e.max(out=m3[:, hd, :], in_=work[1]), r=[K_("work1")], w=[K_("m3")])
                yield
            tt("dve", thr, best[:, :, 15:16], m3[:, :, 0:1], ALU.add, r=[K_("best"), K_("m3")], w=[K_("thr")])
            yield
            ts("dve", thr, thr, 0.5, None, ALU.mult, r=[K_("thr")], w=[K_("thr")])
            yield
            tt("dve", dd, best, best[:, :, 0:1].to_broadcast([128, 8, 16]), ALU.subtract, r=[K_("best")], w=[K_("dd")])
            yield
            act(dd, dd, AF.Exp, r=[K_("dd")], w=[K_("dd")])
            S.op("dve", lambda e: e.tensor_reduce(out=Z, in_=dd, axis=AX.X, op=ALU.add), r=[K_("dd")], w=[K_("Z")])
            yield
            S.op("dve", lambda e: e.reciprocal(out=rZ, in_=Z), r=[K_("Z")], w=[K_("rZ")])
            yield
            s1v = s5[:, tt_, :, 0, :]
            s2v = s5[:, tt_, :, 1, :]
            tt("dve", gthr, thr, best[:, :, 0:1], ALU.subtract, r=[K_("thr"), K_("best")], w=[K_("gthr")])
            yield
            act(gthr, gthr, AF.Exp, r=[K_("gthr")], w=[K_("gthr")])
            tt("dve", gthr, gthr, rZ, ALU.mult, r=[K_("gthr"), K_("rZ")], w=[K_("gthr")])
            yield
            tt("dve", E1q[:, tt_, :, :], s1v, thr[:, :, 0:1].to_broadcast([128, 8, 128]), ALU.subtract,
               r=skeys_ + [K_("thr")], w=[K_("E1q")])
            yield
            act(E1q[:, tt_, :, :], E1q[:, tt_, :, :], AF.Exp, r=[K_("E1q")], w=[K_("E1q")])
            act(E2[:, tt_, :, :], s2v, AF.Exp, r=skeys_, w=[K_("E2")])
            for hd in range(8):
                ts("dve", Dg[:, tt_, hd, :], ident_f, gthr[:, hd, :], None, ALU.mult, r=["ident_f", K_("gthr")], w=[("Dg", tt_, hd)])
                yield

        gens = [tile_gen(0), tile_gen(1)]
        alive = [True, True]
        while any(alive):
            for gi in range(2):
                if alive[gi]:
                    try:
                        next(gens[gi])
                    except StopIteration:
                        alive[gi] = False
        S.barrier()

        LB = Bump(A, lo2, 190 * KB)
        DnT = [LB.alloc([8, 128], BF16) for _ in range(3)]
        Upb = [LB.alloc([D], BF16) for _ in range(4)]
        GA = [LB.alloc([256], BF16) for _ in range(3)]
        mt_ = [LB.alloc([2, 8, 128], F32) for _ in range(2)]
        mb = [LB.alloc([2, 8, 128], BF16) for _ in range(2)]
        WA = [LB.alloc([256], BF16) for _ in range(2)]
        E2f = E2.rearrange("p t h k -> p (t h) k")
        E1qf = E1q.rearrange("p t h k -> p (t h) k")

        def loads(i):
            dma("sp", DnT[i % 3], dnT_d[i], r=[("dnT_d", i)], w=[("DnT", i % 3)])
            dma("sp", Upb[i % 4], up_d[i * 128:(i + 1) * 128, :], r=[("up_d", i)], w=[("Upb", i % 4)])

        loads(0)
        N = NBLK if CUT[0] != 77 else 2
        for it in range(N + 2):
            if it + 1 < N:
                loads(it + 1)
            if it < N:
                i = it
                ND, NP_ = 5, 10
                mt_f = mt_[i % 2].rearrange("p t h k -> p (t h) k")
                tt("dve", mt_f[:, 0:ND, :], E2f[:, 0:ND, :], E1qf[:, 0:ND, i:i + 1].to_broadcast([128, ND, 128]), ALU.mult,
                   r=["E2", "E1q"], w=[("mt", i % 2)])
                tt("pool", mt_f[:, ND:NP_, :], E2f[:, ND:NP_, :], E1qf[:, ND:NP_, i:i + 1].to_broadcast([128, NP_ - ND, 128]), ALU.mult,
                   r=["E2", "E1q"], w=[("mtp", i % 2)])
                for ch in range(NP_, 16):
                    act(mt_f[:, ch, :], E2f[:, ch, :], AF.Copy, r=["E2", "E1q"], w=[("mt1", i % 2, ch)],
                        scale=E1qf[:, ch, i:i + 1])
                b = 4 + i % 2
                for c in range(8):
                    mm(psf(b, 0, 256), DnT[i % 3][:, c, :], hnT[:, c, t0:t0 + 256], c == 0, c == 7,
                       r=[("DnT", i % 3)], w=[("ps", b)])
                act(GA[i % 3], psf(b, 0, 256), AF.Gelu, r=[("ps", b)], w=[("GA", i % 3)])
            if 1 <= it <= N:
                i = it - 1
                stt(mb[i % 2], mt_[i % 2], 1.0, mt_[i % 2], ALU.is_ge, ALU.mult, r=[("mt", i % 2), ("mtp", i % 2)] + [("mt1", i % 2, ch) for ch in range(10, 16)], w=[("mb", i % 2)])
                b = 6 + i % 2
                for tt_ in range(2):
                    for hd in (range(8) if CUT[0] != 78 else [0, 7]):
                        mm(psf(b, tt_ * 128, 128), mb[i % 2][:, tt_, hd, :], Dg[:, tt_, hd, :], hd == 0, hd == 7,
                           r=[("mb", i % 2), "Dg"], w=[("ps", b)])
            if 2 <= it <= N + 1:
                i = it - 2
                b = 6 + i % 2
                tt("dve", WA[i % 2], psf(b, 0, 256), GA[i % 3], ALU.mult, r=[("ps", b), ("GA", i % 3)], w=[("WA", i % 2)])
                for tt_ in range(2):
                    for half in range(2):
                        bo = tt_ * 2 + half
                        mm(psf(bo), WA[i % 2][:, tt_ * 128:(tt_ + 1) * 128], Upb[i % 4][:, half * 512:(half + 1) * 512],
                           i == 0, i == N - 1, r=[("WA", i % 2), ("Upb", i % 4)], w=[("ps", bo)])
        for tt_ in range(2):
            for half in range(2):
                bo = tt_ * 2 + half
                tau = 2 * G + tt_
                tt("dve", h[:, tau, half * 512:(half + 1) * 512], psf(bo), h[:, tau, half * 512:(half + 1) * 512], ALU.add,
                   r=[("ps", bo), ("h", tau)], w=[("h", tau)])
        S.barrier()

    finals = []
    if stage == "full_h2":
        for tau in range(NT):
            finals.append(dma("sp", out[tau * 128:(tau + 1) * 128, :], h[:, tau, :], r=[("h", tau)]))
        S.emit("sp", finals)
        return

    FB = Bump(A, 72 * KB, 190 * KB)
    fin_bc = FB.alloc([D], F32)
    junk = FB.alloc([D], BF16)
    ob = [FB.alloc([D], F32) for _ in range(2)]
    ss = [FB.alloc([1], F32) for _ in range(2)]
    sd = [FB.alloc([1], F32) for _ in range(2)]
    rstd = [FB.alloc([1], F32) for _ in range(2)]
    dma("sp", fin_bc, n_fin.partition_broadcast(128), w=["fin_bc"])
    for tau in range(NT):
        k = tau % 2
        act(junk, h[:, tau, :], AF.Square, r=[("h", tau)], w=["fjunk", ("fss", k)], accum_out=ss[k])
        act(sd[k], ss[k], AF.Sqrt, r=[("fss", k)], w=[("fsd", k)], scale=1.0 / D, bias=1e-6)
        S.op("dve", lambda e, k=k: e.reciprocal(out=rstd[k], in_=sd[k]), r=[("fsd", k)], w=[("frstd", k)])
        stt(ob[k], h[:, tau, :], rstd[k], fin_bc, ALU.mult, ALU.mult, r=[("h", tau), ("frstd", k), "fin_bc"], w=[("ob", k)])
        finals.append(dma("sp", out[tau * 128:(tau + 1) * 128, :], ob[k], r=[("ob", k)]))
    S.emit("sp", finals)


def make_consts():
    c = {}
    c["c_ident"] = np.eye(128, dtype=np.float32)
    k = np.arange(128)[:, None]
    q = np.arange(128)[None, :]
    c["c_mask"] = np.concatenate([(k >= q), (k <= q)], axis=1).astype(np.float32)
    e1 = np.zeros((128, 516), np.float32)
    e1[np.arange(128), 4 + 4 * np.arange(128)] = 1.0
    e2 = np.zeros((128, 2064), np.float32)
    e2[np.arange(128), 16 + 16 * np.arange(128)] = 1.0
    c["c_e1"], c["c_e2"] = e1, e2
    inv_freq = (np.float32(500000.0) ** (-np.arange(8, dtype=np.float32) * np.float32(2.0 / 16))).astype(np.float32)
    rope = np.zeros((128, 2), np.float32)
    for p in range(128):
        d = p % 64
        if d < 16:
            rope[p, 0] = inv_freq[d % 8]
            rope[p, 1] = -1.0 if d < 8 else 1.0
    c["c_rope"] = rope
    return c


def make_in_maps(inputs, stage="full"):
    x = np.asarray(inputs["x"], np.float32)
    positions = np.asarray(inputs["positions"], np.int32)
    consts = make_consts()
    shared = {
        "w_in": np.ascontiguousarray(inputs["w_in"][0]),
        "conv_w": np.ascontiguousarray(inputs["conv_w"][0]),
        "w_conv_out": np.ascontiguousarray(inputs["w_conv_out"][0]),
        "w_attn_out": np.ascontiguousarray(inputs["w_attn_out"][0]),
        "gate_bias": np.ascontiguousarray(inputs["gate_bias"][0]),
        "w_out": np.ascontiguousarray(inputs["w_out"][0]),
        "norm_mix": np.ascontiguousarray(inputs["norm_mix"][0]),
        "norm_ffn": np.ascontiguousarray(inputs["norm_ffn"][0]),
        "final_norm": np.ascontiguousarray(inputs["final_norm"]),
        "peer_w_query": np.ascontiguousarray(inputs["peer_w_query"][0]),
        "peer_sub_keys": np.ascontiguousarray(inputs["peer_sub_keys"][0]).reshape(16, 128, 128),
        "peer_down": np.ascontiguousarray(inputs["peer_down"][0]),
        "peer_up": np.ascontiguousarray(inputs["peer_up"][0]),
    }
    shared = {k: np.asarray(v, np.float32) for k, v in shared.items()}
    shared.update(consts)
    if stage not in ("full", "full_h2"):
        shared.pop("peer_down"); shared.pop("peer_up")
    maps = []
    for core in range(8):
        b, q = core // 4, core % 4
        m = dict(shared)
        m["x_own"] = np.ascontiguousarray(x[b, q * T:(q + 1) * T])
        if q > 0:
            m["x_halo"] = np.ascontiguousarray(x[b, (q - 1) * T:q * T])
            ph = positions[b, (q - 1) * T:q * T]
        else:
            m["x_halo"] = np.zeros((T, D), np.float32)
            ph = np.zeros((T,), np.int32)
        m["pos"] = np.ascontiguousarray(np.concatenate([ph, positions[b, q * T:(q + 1) * T]])[None, :]).astype(np.int32)
        m["halo_flag"] = np.full((128, 1), 1.0 if q > 0 else 0.0, np.float32)
        maps.append(m)
    return maps


_CACHE = {}


def kernel(**inputs):
    stage = inputs.pop("_stage", "full")
    if stage not in _CACHE:
        _CACHE[stage] = build(stage)
    nc, _ = _CACHE[stage]
    maps = make_in_maps(inputs, stage)
    res = run_bass_kernel_spmd(nc, maps, core_ids=list(range(8)))
    outs = [np.asarray(r["out"], np.float32) for r in res.results]
    full = np.stack([np.concatenate(outs[0:4], axis=0), np.concatenate(outs[4:8], axis=0)], axis=0)
    return full.astype(np.float32)
```

```python
import numpy as np
import concourse.bass as bass
import concourse.mybir as mybir
from concourse.bass_utils import run_bass_kernel_spmd

F32 = mybir.dt.float32
BF16 = mybir.dt.bfloat16
I32 = mybir.dt.int32
U8 = mybir.dt.uint8
AF = mybir.ActivationFunctionType
ALU = mybir.AluOpType
AX = mybir.AxisListType

ENGS = ["pe", "act", "dve", "pool", "sp"]
NDMASEM = 8


class Sched:
    def __init__(self, nc):
        self.nc = nc
        self.ops = {e: [] for e in ENGS}
        self.last_w = {}
        self.readers = {}
        self.dma_count = {"sp": 0, "pool": 0, "act": 0}
        self.same_engine_sync = {"pe": False, "act": True, "dve": True, "pool": True, "sp": False}
        self.global_deps = []
        self.open_dmas = []

    def barrier(self):
        toks = []
        for e in ENGS:
            for i in range(len(self.ops[e]) - 1, -1, -1):
                if self.ops[e][i]["dma"] is None:
                    toks.append(("e", e, i))
                    break
        toks.extend(self.open_dmas)
        self.open_dmas = []
        self.global_deps = toks

    def _deps(self, reads, writes):
        deps = []
        for r in reads:
            t = self.last_w.get(r)
            if t is not None:
                deps.append(t)
        for w in writes:
            t = self.last_w.get(w)
            if t is not None:
                deps.append(t)
            deps.extend(self.readers.get(w, ()))
        return deps

    def _commit(self, tok, reads, writes):
        for r in reads:
            self.readers.setdefault(r, []).append(tok)
        for w in writes:
            self.last_w[w] = tok
            self.readers[w] = []

    @staticmethod
    def _excl(r, w):
        r2 = [k for k in r if not (isinstance(k, tuple) and k[0] == "ps")]
        w2 = list(w) + [k for k in r if isinstance(k, tuple) and k[0] == "ps"]
        return r2, w2

    def op(self, eng, fn, r=(), w=()):
        r, w = self._excl(r, w)
        deps = self._deps(r, w)
        idx = len(self.ops[eng])
        waits = []
        for t in list(deps) + self.global_deps:
            if t[0] == "e" and t[1] == eng and not self.same_engine_sync[eng]:
                continue
            waits.append(t)
        self.ops[eng].append(dict(fn=fn, waits=waits, dma=None))
        tok = ("e", eng, idx)
        self._commit(tok, r, w)
        return tok

    def dma(self, q, fn, r=(), w=()):
        r, w = self._excl(r, w)
        deps = self._deps(r, w)
        n = self.dma_count[q]
        self.dma_count[q] += 1
        slot, gen = n % NDMASEM, n // NDMASEM
        waits = list(deps) + [t for t in self.global_deps if not (t[0] == "e" and t[1] == q and not self.same_engine_sync[q])]
        if gen > 0:
            waits.append(("d", q, slot, gen))
        self.ops[q].append(dict(fn=fn, waits=waits, dma=(q, slot)))
        tok = ("d", q, slot, gen + 1)
        self.open_dmas.append(tok)
        self._commit(tok, r, w)
        return tok

    def emit(self, final_waits_engine="sp", final_toks=()):
        nc = self.nc
        needed = {e: set() for e in ENGS}
        for e in ENGS:
            for o in self.ops[e]:
                for t in o["waits"]:
                    if t[0] == "e":
                        needed[t[1]].add(t[2])
        for t in final_toks:
            if t[0] == "e":
                needed[t[1]].add(t[2])
        cnt = {}
        for e in ENGS:
            c = 0
            m = {}
            for i in range(len(self.ops[e])):
                if i in needed[e]:
                    c += 1
                    m[i] = c
            cnt[e] = m
        self.max_sem = {e: (max(cnt[e].values()) if cnt[e] else 0) for e in ENGS}
        esem = {e: nc.alloc_semaphore(name=f"prog_{e}") for e in ENGS}
        dsem = {q: [nc.alloc_semaphore(name=f"dma_{q}_{i}") for i in range(NDMASEM)]
                for q in ("sp", "pool", "act")}
        handles = {"pe": "tensor", "act": "scalar", "dve": "vector", "pool": "gpsimd", "sp": "sync"}

        def tokval(t):
            if t[0] == "e":
                return esem[t[1]], cnt[t[1]][t[2]]
            return dsem[t[1]][t[2]], 16 * t[3]

        def run_engine(ename, eng):
            waited = {}
            for i, o in enumerate(self.ops[ename]):
                for t in o["waits"]:
                    s, v = tokval(t)
                    key = id(s)
                    if waited.get(key, 0) >= v:
                        continue
                    eng.wait_ge(s, v)
                    waited[key] = v
                inst = o["fn"](eng)
                if o["dma"] is not None:
                    q, slot = o["dma"]
                    inst.then_inc(dsem[q][slot], 16)
                elif i in needed[ename]:
                    inst.then_inc(esem[ename], 1)
            if ename == final_waits_engine:
                for t in final_toks:
                    s, v = tokval(t)
                    eng.wait_ge(s, v)

        with nc.Block() as block:
            for ename in ENGS:
                deco = getattr(block, handles[ename])

                def body(eng, ename=ename):
                    run_engine(ename, eng)
                deco(body)


class Arena:
    def __init__(self, nc, nbytes, name="arena"):
        self.t = nc.alloc_sbuf_tensor(name, [128, nbytes], U8)
        self.n = nbytes
        self.top = 0

    def mark(self):
        return self.top

    def release(self, m):
        self.top = m

    def alloc(self, shape_free, dtype, parts=128):
        esz = {F32: 4, BF16: 2, I32: 4}[dtype]
        n = int(np.prod(shape_free)) * esz
        off = (self.top + 63) // 64 * 64
        assert off + n <= self.n, f"arena overflow {off}+{n}>{self.n}"
        self.top = off + n
        ap = self.t[0:parts, off:off + n].bitcast(dtype)
        if len(shape_free) > 1:
            names = " ".join(f"d{i}" for i in range(len(shape_free)))
            kw = {f"d{i}": int(s) for i, s in enumerate(shape_free)}
            ap = ap.rearrange(f"p ({names}) -> p {names}", **kw)
        return ap

    def view(self, off, shape_free, dtype, parts=128):
        esz = {F32: 4, BF16: 2, I32: 4}[dtype]
        n = int(np.prod(shape_free)) * esz
        assert off % 4 == 0 and off + n <= self.n, f"arena overflow {off}+{n}>{self.n}"
        ap = self.t[0:parts, off:off + n].bitcast(dtype)
        if len(shape_free) > 1:
            names = " ".join(f"d{i}" for i in range(len(shape_free)))
            kw = {f"d{i}": int(s) for i, s in enumerate(shape_free)}
            ap = ap.rearrange(f"p ({names}) -> p {names}", **kw)
        return ap


class Bump:
    def __init__(self, arena, lo, hi):
        self.a, self.lo, self.hi, self.top = arena, lo, hi, lo

    def alloc(self, shape_free, dtype, parts=128):
        esz = {F32: 4, BF16: 2, I32: 4}[dtype]
        n = int(np.prod(shape_free)) * esz
        off = (self.top + 63) // 64 * 64
        assert off + n <= self.hi, f"bump overflow {off}+{n}>{self.hi}"
        self.top = off + n
        return self.a.view(off, shape_free, dtype, parts)


KB = 1024
CUT = [0]
VBANK = [7]
D = 1024
T = 2048
NT = 16
C_B, C_C, C_X, C_Q, C_K, C_V, C_GC, C_GA = 0, 1024, 2048, 3072, 4608, 6144, 7680, 8704
GROUPS = [(1, 128), (4, 512), (16, 2048)]
MAGIC = 12582912.0
TWO_PI = 6.283185307179586
CW1 = 6.28125
CW2 = TWO_PI - CW1
PI_S = 3.1415925


def build(stage="full"):
    nc = bass.Bass("TRN2", target_bir_lowering=False)

    def din(name, shape, dt=F32):
        return nc.dram_tensor(name, list(shape), dt, kind="ExternalInput").ap()

    x_own = din("x_own", [T, D])
    x_halo = din("x_halo", [T, D])
    pos = din("pos", [1, 2 * T], I32)
    flag_d = din("halo_flag", [128, 1])
    w_in = din("w_in", [D, 9728])
    conv_w = din("conv_w", [3, D])
    w_co = din("w_conv_out", [D, D])
    w_ao = din("w_attn_out", [512, D])
    gbias = din("gate_bias", [2, D])
    w_out = din("w_out", [D, D])
    n_mix = din("norm_mix", [D])
    n_ffn = din("norm_ffn", [D])
    n_fin = din("final_norm", [D])
    wqry = din("peer_w_query", [D, 2048])
    skeys = din("peer_sub_keys", [16, 128, 128])
    if stage in ("full", "full_h2"):
        pdown = din("peer_down", [16384, D])
        pup = din("peer_up", [16384, D])
    c_ident = din("c_ident", [128, 128])
    c_mask = din("c_mask", [128, 256])
    c_e1 = din("c_e1", [128, 516])
    c_e2 = din("c_e2", [128, 2064])
    c_rope = din("c_rope", [128, 2])
    out = nc.dram_tensor("out", [T, D], F32, kind="ExternalOutput").ap()

    S = Sched(nc)
    A = Arena(nc, 206 * KB)
    ps = [nc.alloc_psum_tensor(f"psb{i}", [128, 512], F32) for i in range(8)]

    def psf(b, c0=0, n=512):
        return ps[b][:, c0:c0 + n]

    def psb(b):
        return ps[b][:, :].bitcast(BF16)

    w_in_v = w_in.rearrange("(c p) n -> p c n", p=128)
    w_co_v = w_co.rearrange("(c p) n -> p c n", p=128)
    w_ao_v = w_ao.rearrange("(c p) n -> p c n", p=128)
    w_out_v = w_out.rearrange("(c p) n -> p c n", p=128)
    wqry_v = wqry.rearrange("(c p) n -> p c n", p=128)

    CB = Bump(A, 0, 8 * KB)
    ident_f = CB.alloc([128], F32)
    ident_b = CB.alloc([128], BF16)
    mask_b = CB.alloc([256], BF16)
    e1_b = CB.alloc([516], BF16)
    e2_b = CB.alloc([2064], BF16)
    rope = CB.alloc([2], F32)
    flag = CB.alloc([1], F32)
    gmixT = CB.alloc([8, 1], F32)
    gffnT = CB.alloc([8, 1], F32)
    convwT = CB.alloc([3, 8, 1], F32)
    gbT = CB.alloc([2, 8, 1], F32)

    def dma(q, out_ap, in_ap, r=(), w=(), slow=False):
        def fn(e, out_ap=out_ap, in_ap=in_ap):
            if slow:
                return e.dma_start(out=out_ap, in_=in_ap, allow_slow_non_contiguous=True)
            return e.dma_start(out=out_ap, in_=in_ap)
        return S.dma(q, fn, r=r, w=w)

    WST = [A.view(190 * KB + i * 4096, [8, 128], F32) for i in range(4)]
    wst_n = [0]

    def cast_load(dst, src, ncols_total, dkey, src_is_w=True):
        kc = dst.shape[1]
        for c0 in range(0, ncols_total, 128):
            n = min(128, ncols_total - c0)
            i = wst_n[0] % 4
            wst_n[0] += 1
            st = WST[i][:, 0:kc, 0:n]
            dma("sp", st, src[:, :, c0:c0 + n], w=[("wst", i)])
            S.op("pool", lambda e, st=st, d=dst[:, :, c0:c0 + n]: e.tensor_copy(out=d, in_=st),
                 r=[("wst", i)], w=[dkey])

    def cast_load2(dst, src, dkey):
        n = dst.shape[1]
        for c0 in range(0, n, 1024):
            m = min(1024, n - c0)
            i = wst_n[0] % 4
            wst_n[0] += 1
            st = WST[i].rearrange("p a b -> p (a b)")[:, 0:m]
            dma("sp", st, src[:, c0:c0 + m], w=[("wst", i)])
            S.op("pool", lambda e, st=st, d=dst[:, c0:c0 + m]: e.tensor_copy(out=d, in_=st),
                 r=[("wst", i)], w=[dkey])

    dma("sp", ident_f, c_ident, w=["ident_f"])
    cast_load2(ident_b, c_ident, "ident_b")
    cast_load2(mask_b, c_mask, "mask_b")
    cast_load2(e1_b, c_e1, "e1_b")
    cast_load2(e2_b, c_e2, "e2_b")
    dma("sp", rope, c_rope, w=["rope"])
    dma("sp", flag, flag_d, w=["flag"])
    dma("sp", gmixT, n_mix.rearrange("(c p o) -> p c o", p=128, o=1), w=["gmixT"], slow=True)
    dma("sp", gffnT, n_ffn.rearrange("(c p o) -> p c o", p=128, o=1), w=["gffnT"], slow=True)
    for i_ in range(3):
        dma("sp", convwT[:, i_, :, :], conv_w[i_, :].rearrange("(c p o) -> p c o", p=128, o=1), w=["convwT"], slow=True)
    for j_ in range(2):
        dma("sp", gbT[:, j_, :, :], gbias[j_, :].rearrange("(c p o) -> p c o", p=128, o=1), w=["gbT"], slow=True)

    def early_exit(off):
        S.barrier()
        src = A.view(off, [NT, D], F32)
        fin = [dma("sp", out.rearrange("(t p) d -> p t d", p=128), src)]
        S.emit("sp", fin)
        return nc, S

    if stage == "const":
        return early_exit(0)

    def mm(out_ap, lhsT, rhs, start, stop, r=(), w=()):
        return S.op("pe", lambda e: e.matmul(out_ap, lhsT=lhsT, rhs=rhs, start=start, stop=stop), r=r, w=w)

    def tr(out_ap, in_ap, ident, r=(), w=()):
        return S.op("pe", lambda e: e.transpose(out_ap, in_ap, ident), r=r, w=w)

    def act(out_ap, in_ap, func, r=(), w=(), **kw):
        return S.op("act", lambda e: e.activation(out=out_ap, in_=in_ap, func=func, **kw), r=r, w=w)

    def tt(eng, out_ap, in0, in1, op, r=(), w=()):
        return S.op(eng, lambda e: e.tensor_tensor(out=out_ap, in0=in0, in1=in1, op=op), r=r, w=w)

    def ts(eng, out_ap, in0, s1, s2, op0, op1=None, r=(), w=()):
        if op1 is None:
            return S.op(eng, lambda e: e.tensor_scalar(out=out_ap, in0=in0, scalar1=s1, scalar2=None, op0=op0), r=r, w=w)
        return S.op(eng, lambda e: e.tensor_scalar(out=out_ap, in0=in0, scalar1=s1, scalar2=s2, op0=op0, op1=op1), r=r, w=w)

    def stt(out_ap, in0, scalar, in1, op0, op1, r=(), w=()):
        return S.op("dve", lambda e: e.scalar_tensor_tensor(out=out_ap, in0=in0, scalar=scalar, in1=in1, op0=op0, op1=op1), r=r, w=w)

    def cp(eng, out_ap, in_ap, r=(), w=()):
        return S.op(eng, lambda e: e.tensor_copy(out=out_ap, in_=in_ap), r=r, w=w)

    def xkeys(t0, n, step=1):
        return [("xT", i) for i in range(t0 // 128, (t0 + (n - 1) * step) // 128 + 1)]

    def load_w(dst, src_v, c0, ncols, key):
        return cast_load(dst, src_v[:, :, c0:c0 + ncols], ncols, key)

    def proj_fm(ps_ap, pskey, wt, wkey, xT, t0, n, kc=8):
        for c in range(kc):
            mm(ps_ap, wt[:, c, :], xT[:, c, t0:t0 + n], c == 0, c == kc - 1,
               r=[wkey] + xkeys(t0, n), w=[pskey])

    xT_all = A.view(8 * KB, [8, 2 * T], BF16)
    cosT = A.view(72 * KB, [2 * T], F32)
    sinT = A.view(88 * KB, [2 * T], F32)
    oT = A.view(104 * KB, [4, T], BF16)

    TB = Bump(A, 104 * KB, 190 * KB)
    posi = TB.alloc([2 * T], I32)
    ang = TB.alloc([2 * T], F32)
    uu = TB.alloc([2 * T], F32)
    kf = TB.alloc([2 * T], F32)
    rr = TB.alloc([2 * T], F32)
    dma("sp", posi, pos[0, :].partition_broadcast(128), w=["posi"])
    cp("dve", ang, posi, r=["posi"], w=["ang"])
    ts("dve", ang, ang, rope[:, 0:1], None, ALU.mult, r=["ang", "rope"], w=["ang"])

    def sin_table(dst, dkey, shift, scale):
        ts("dve", uu, ang, float(shift), None, ALU.add, r=["ang"], w=["uu"])
        ts("dve", kf, uu, 1.0 / TWO_PI, MAGIC, ALU.mult, ALU.add, r=["uu"], w=["kf"])
        ts("dve", kf, kf, -MAGIC, None, ALU.add, r=["kf"], w=["kf"])
        stt(rr, kf, -CW1, uu, ALU.mult, ALU.add, r=["kf", "uu"], w=["rr"])
        stt(rr, kf, -CW2, rr, ALU.mult, ALU.add, r=["kf", "rr"], w=["rr"])
        ts("dve", rr, rr, -PI_S, PI_S, ALU.max, ALU.min, r=["rr"], w=["rr"])
        act(dst, rr, AF.Sin, r=["rr", "rope"], w=[dkey], scale=scale)

    sin_table(cosT, "cosT", np.pi / 2, 1.0)
    sin_table(sinT, "sinT", 0.0, rope[:, 1:2])
    S.barrier()
    if stage == "rope":
        return early_exit(72 * KB)

    def norm_phase(src_tiles, gT, dstT, dkey, B, tile0=0):
        xin = [B.alloc([D], F32) for _ in range(2)]
        xs = [B.alloc([D], BF16) for _ in range(2)]
        junk = B.alloc([D], BF16)
        ss = [B.alloc([1], F32) for _ in range(2)]
        sd = [B.alloc([1], F32) for _ in range(2)]
        rstd = [B.alloc([1], F32) for _ in range(2)]
        for i, src in enumerate(src_tiles):
            k = i % 2
            if src[0] == "dram":
                dma("sp", xin[k], src[1], w=[("xin", k)])
                xi, xkey = xin[k], ("xin", k)
            else:
                xi, xkey = src[1], src[2]
            if CUT[0] == 9:
                continue
            act(junk, xi, AF.Square, r=[xkey], w=["junk", ("ss", k)], accum_out=ss[k])
            if CUT[0] == 1:
                continue
            act(sd[k], ss[k], AF.Sqrt, r=[("ss", k)], w=[("sd", k)], scale=1.0 / D, bias=1e-6)
            if CUT[0] == 2:
                continue
            S.op("dve", lambda e, k=k: e.reciprocal(out=rstd[k], in_=sd[k]), r=[("sd", k)], w=[("rstd", k)])
            if CUT[0] == 3:
                continue
            act(xs[k], xi, AF.Copy, r=[xkey, ("rstd", k)], w=[("xs", k)], scale=rstd[k])
            if CUT[0] == 4:
                continue
            pb = psb(k)
            for c in range(8):
                tr(pb[:, c * 128:(c + 1) * 128], xs[k][:, c * 128:(c + 1) * 128], ident_b,
                   r=[("xs", k), "ident_b"], w=[("ps", k)])
            if CUT[0] == 5:
                continue
            tt("dve", dstT[:, :, (tile0 + i) * 128:(tile0 + i + 1) * 128],
               pb.rearrange("p (c t) -> p c t", c=8), gT[:, :, 0:1].to_broadcast([128, 8, 128]), ALU.mult,
               r=[("ps", k)], w=[(dkey, tile0 + i)])

    PB = Bump(A, 120 * KB, 190 * KB)
    tiles = [("dram", x_halo[i * 128:(i + 1) * 128, :]) for i in range(NT)] + \
            [("dram", x_own[i * 128:(i + 1) * 128, :]) for i in range(NT)]
    if stage.startswith("A") and len(stage) > 1:
        tiles = tiles[:int(stage[1:])]
    norm_phase(tiles, gmixT, xT_all, "xT", PB)
    S.barrier()
    if stage.startswith("A"):
        return early_exit(8 * KB)

    AB = Bump(A, 120 * KB, 190 * KB)
    wq_t = [AB.alloc([8, 128], BF16) for _ in range(2)]
    wqs_t = [AB.alloc([8, 128], BF16) for _ in range(2)]
    wk_t = [AB.alloc([8, 128], BF16) for _ in range(2)]
    wks_t = [AB.alloc([8, 128], BF16) for _ in range(2)]
    wv_t = [AB.alloc([8, 128], BF16) for _ in range(2)]
    Qbuf = AB.alloc([T], BF16)
    Kbuf = AB.alloc([2 * T], BF16)
    Vbuf = AB.alloc([32, 2, 66], BF16)
    U = [AB.alloc([16, 130], BF16) for _ in range(3)]
    tA = [AB.alloc([512], F32) for _ in range(2)]
    tB = [AB.alloc([512], F32) for _ in range(2)]
    Pt = [AB.alloc([256], BF16) for _ in range(4)]
    otok = [AB.alloc([2, 64], BF16) for _ in range(2)]
    rl = [AB.alloc([2, 1], F32) for _ in range(2)]
    for k in range(2):
        S.op("pool", lambda e, k=k: e.memset(wqs_t[k], 0.0), w=[("wqs", k)])
        S.op("pool", lambda e, k=k: e.memset(wks_t[k], 0.0), w=[("wks", k)])

    def build_sw(dst, dkey, src, skey):
        for hh in range(2):
            b = hh * 64
            cp("pool", dst[:, :, b:b + 8], src[:, :, b + 8:b + 16], r=[skey], w=[dkey])
            cp("pool", dst[:, :, b + 8:b + 16], src[:, :, b:b + 8], r=[skey], w=[dkey])

    def perm_out(buf, nblk, dil, span, lt0, cs):
        v4 = buf[:, 0:nblk * span].rearrange("p (n r i) -> p n i r", n=nblk, r=dil, i=128)
        if cs >= span:
            n0, nn = lt0 // span, cs // span
            return v4[:, n0:n0 + nn, :, :], (lambda a: a[:, 0:cs].rearrange("p (n i r) -> p n i r", n=nn, i=128, r=dil))
        n0 = lt0 // span
        i0, ni = (lt0 % span) // dil, cs // dil
        return v4[:, n0, i0:i0 + ni, :], (lambda a: a[:, 0:cs].rearrange("p (i r) -> p i r", r=dil))

    wcnt = [0]
    pcnt = [0]
    for pp in range(4):
        for g, (dil, span) in enumerate(GROUPS):
            hp = g * 4 + pp
            nb = T // span
            k0 = T - span
            nk = span + T
            wi = wcnt[0] % 2
            wcnt[0] += 1
            load_w(wq_t[wi], w_in_v, C_Q + hp * 128, 128, ("wq", wi))
            build_sw(wqs_t[wi], ("wqs", wi), wq_t[wi], ("wq", wi))
            load_w(wk_t[wi], w_in_v, C_K + hp * 128, 128, ("wk", wi))
            build_sw(wks_t[wi], ("wks", wi), wk_t[wi], ("wk", wi))
            load_w(wv_t[wi], w_in_v, C_V + hp * 128, 128, ("wv", wi))

            def rot_chunk(wt, wkey, wst, wskey, t0, cs, buf, bkey, nblk, lt0):
                i = pcnt[0] % 2
                pcnt[0] += 1
                b0 = i * 2
                proj_fm(psf(b0, 0, cs), ("ps", b0), wt, wkey, xT_all, t0, cs)
                proj_fm(psf(b0 + 1, 0, cs), ("ps", b0 + 1), wst, wskey, xT_all, t0, cs)
                tt("dve", tA[i][:, 0:cs], psf(b0, 0, cs), cosT[:, t0:t0 + cs], ALU.mult,
                   r=[("ps", b0), "cosT"], w=[("tA", i)])
                tt("dve", tB[i][:, 0:cs], psf(b0 + 1, 0, cs), sinT[:, t0:t0 + cs], ALU.mult,
                   r=[("ps", b0 + 1), "sinT"], w=[("tB", i)])
                ov, inr = perm_out(buf, nblk, dil, span, lt0, cs)
                tt("dve", ov, inr(tA[i]), inr(tB[i]), ALU.add, r=[("tA", i), ("tB", i)], w=[bkey])

            for j in range(4):
                rot_chunk(wq_t[wi], ("wq", wi), wqs_t[wi], ("wqs", wi), T + j * 512, 512, Qbuf, "Q", nb, j * 512)
            if stage == "att1":
                return early_exit(120 * KB)
            lt = 0
            while lt < nk:
                cs = min(512, nk - lt)
                rot_chunk(wk_t[wi], ("wk", wi), wks_t[wi], ("wks", wi), k0 + lt, cs, Kbuf, "K", nb + 1, lt)
                lt += cs
            if stage == "att2":
                return early_exit(120 * KB)
            nvb = (nb + 1) * dil
            S.op("dve", lambda e, nvb=nvb: e.memset(Vbuf[:, 0:nvb, :, 64:65], 1.0), w=["V"])
            ts("dve", Vbuf[:, 0:dil, :, 64:65], Vbuf[:, 0:dil, :, 64:65], flag[:, 0:1], None, ALU.mult,
               r=["flag"], w=["V"])
            if stage == "att3a":
                return early_exit(120 * KB)
            for blk in range(nvb):
                n_, r_ = blk // dil, blk % dil
                st = k0 + n_ * span + r_
                s4 = 6 + blk % 2
                pv = psf(s4, 0, 128)
                for c in range(8):
                    mm(pv, xT_all[:, c, st:st + 127 * dil + 1:dil], wv_t[wi][:, c, :], c == 0, c == 7,
                       r=[("wv", wi)] + xkeys(st, 128, dil), w=[("ps", s4)])
                if stage == "att3b":
                    continue
                for hh_ in range(2):
                    cp("dve", Vbuf[:, blk, hh_, 0:64], pv[:, hh_ * 64:(hh_ + 1) * 64],
                       r=[("ps", s4)], w=["V"])
            if stage in ("att3", "att3b"):
                return early_exit(120 * KB)
            Kb = Kbuf[:, 0:nk].rearrange("p (n r i) -> p n r i", n=nb + 1, r=dil, i=128)
            Qb = Qbuf[:, 0:T].rearrange("p (n r i) -> p n r i", n=nb, r=dil, i=128)
            Ug = U[g].rearrange("p b (h d) -> p b h d", h=2)
            items = [(b16, hh) for b16 in range(16) for hh in range(2)]

            def st_scores(ii, b16, hh):
                n_, r_ = b16 // dil, b16 % dil
                rows = slice(hh * 64, hh * 64 + 64)
                s2 = 4 + ii % 2
                pS = psf(s2, 0, 256)
                mm(pS[:, 0:128], Kb[rows, n_, r_, :], Qb[rows, n_, r_, :], True, True, r=["K", "Q"], w=[("ps", s2)])
                mm(pS[:, 128:256], Kb[rows, n_ + 1, r_, :], Qb[rows, n_, r_, :], True, True, r=["K", "Q"], w=[("ps", s2)])
                p4 = ii % 4
                act(Pt[p4], pS, AF.Exp, r=[("ps", s2)], w=[("Pt", p4)], scale=0.125)
                tt("dve", Pt[p4], Pt[p4], mask_b, ALU.mult, r=[("Pt", p4), "mask_b"], w=[("Pt", p4)])

            def st_pv(ii, b16, hh):
                n_, r_ = b16 // dil, b16 % dil
                p4 = ii % 4
                b6 = 6 + ii % 2
                pO = psf(b6, 0, 65)
                mm(pO, Pt[p4][:, 0:128], Vbuf[:, n_ * dil + r_, hh, 0:65], True, False, r=[("Pt", p4), "V"], w=[("ps", b6)])
                mm(pO, Pt[p4][:, 128:256], Vbuf[:, (n_ + 1) * dil + r_, hh, 0:65], False, True, r=[("Pt", p4), "V"], w=[("ps", b6)])
                cp("dve", Ug[:, b16, hh, :], pO, r=[("ps", b6)], w=[("U", g)])

            for ii in range(len(items) + 1):
                if ii < len(items):
                    st_scores(ii, *items[ii])
                if ii >= 1:
                    st_pv(ii - 1, *items[ii - 1])
            if stage == "att4":
                return early_exit(120 * KB)
        for tau in range(NT):
            c2 = tau % 2
            pC = psf(c2, 0, 130)
            ops = [(ident_b, U[0][:, tau, :], ("U", 0))]
            for r_ in range(4):
                off = 4 - r_ + (tau % 4) * 128
                ops.append((e1_b[:, off:off + 128], U[1][:, (tau // 4) * 4 + r_, :], ("U", 1)))
            for r_ in range(16):
                off = 16 - r_ + tau * 128
                ops.append((e2_b[:, off:off + 128], U[2][:, r_, :], ("U", 2)))
            for oi, (l_, r_ap, uk) in enumerate(ops):
                mm(pC, l_, r_ap, oi == 0, oi == len(ops) - 1, r=[uk, "ident_b", "e1_b", "e2_b"], w=[("ps", c2)])
            pC3 = pC.rearrange("p (h d) -> p h d", h=2)
            S.op("dve", lambda e, c2=c2, pC3=pC3: e.reciprocal(out=rl[c2], in_=pC3[:, :, 64:65]),
                 r=[("ps", c2)], w=[("rl", c2)])
            tt("dve", otok[c2], pC3[:, :, 0:64], rl[c2][:, :, 0:1].to_broadcast([128, 2, 64]), ALU.mult,
               r=[("ps", c2), ("rl", c2)], w=[("otok", c2)])
            pT = psb(2 + c2)[:, 0:128]
            tr(pT, otok[c2].rearrange("p h d -> p (h d)"), ident_b, r=[("otok", c2), "ident_b"], w=[("ps", 2 + c2)])
            act(oT[:, pp, tau * 128:(tau + 1) * 128], pT, AF.Copy, r=[("ps", 2 + c2)], w=["oT"])
    S.barrier()

    uT = A.view(120 * KB, [8, T], BF16)
    VB = Bump(A, 152 * KB, 190 * KB)
    zbuf = VB.alloc([T + 2], F32)
    acc = VB.alloc([T], F32)
    ctmp = [VB.alloc([512], F32) for _ in range(2)]
    wb_t = [VB.alloc([8, 128], BF16) for _ in range(2)]
    wc_t = [VB.alloc([8, 128], BF16) for _ in range(2)]
    wx_t = [VB.alloc([8, 128], BF16) for _ in range(2)]
    pc = [0]
    for ft in range(8):
        wi = ft % 2
        load_w(wc_t[wi], w_in_v, C_C + ft * 128, 128, ("wc", wi))
        load_w(wx_t[wi], w_in_v, C_X + ft * 128, 128, ("wx", wi))
        load_w(wb_t[wi], w_in_v, C_B + ft * 128, 128, ("wb", wi))
        chunks = [(T - 2, 2, 0)] + [(T + j * 512, 512, 2 + j * 512) for j in range(4)]
        for (t0, cs, z0) in chunks:
            i = pc[0] % 2
            pc[0] += 1
            b0 = i * 2
            proj_fm(psf(b0, 0, cs), ("ps", b0), wc_t[wi], ("wc", wi), xT_all, t0, cs)
            proj_fm(psf(b0 + 1, 0, cs), ("ps", b0 + 1), wx_t[wi], ("wx", wi), xT_all, t0, cs)
            act(ctmp[i][:, 0:cs], psf(b0, 0, cs), AF.Copy, r=[("ps", b0)], w=[("ctmp", i)])
            tt("dve", zbuf[:, z0:z0 + cs], ctmp[i][:, 0:cs], psf(b0 + 1, 0, cs), ALU.mult,
               r=[("ctmp", i), ("ps", b0 + 1)], w=["z"])
        ts("dve", acc, zbuf[:, 0:T], convwT[:, 0, ft, :], None, ALU.mult, r=["z", "convwT"], w=["acc"])
        stt(acc, zbuf[:, 1:T + 1], convwT[:, 1, ft, :], acc, ALU.mult, ALU.add, r=["z", "acc"], w=["acc"])
        stt(acc, zbuf[:, 2:T + 2], convwT[:, 2, ft, :], acc, ALU.mult, ALU.add, r=["z", "acc"], w=["acc"])
        for j in range(4):
            i = pc[0] % 2
            pc[0] += 1
            b0 = i * 2
            proj_fm(psf(b0), ("ps", b0), wb_t[wi], ("wb", wi), xT_all, T + j * 512, 512)
            tt("dve", uT[:, ft, j * 512:(j + 1) * 512], acc[:, j * 512:(j + 1) * 512], psf(b0), ALU.mult,
               r=["acc", ("ps", b0)], w=["uT"])
    S.barrier()

    mergedT = A.view(72 * KB, [8, T], BF16)
    MB = Bump(A, 152 * KB, 190 * KB)
    wco_t = [MB.alloc([8, 128], BF16) for _ in range(2)]
    wgc_t = [MB.alloc([8, 128], BF16) for _ in range(2)]
    wao_t = [MB.alloc([4, 128], BF16) for _ in range(2)]
    wga_t = [MB.alloc([8, 128], BF16) for _ in range(2)]
    sg = [MB.alloc([512], F32) for _ in range(2)]
    sga = [MB.alloc([512], F32) for _ in range(2)]
    m1 = [MB.alloc([512], F32) for _ in range(2)]
    m2 = [MB.alloc([512], F32) for _ in range(2)]
    for mt in range(8):
        wi = mt % 2
        load_w(wco_t[wi], w_co_v, mt * 128, 128, ("wco", wi))
        load_w(wgc_t[wi], w_in_v, C_GC + mt * 128, 128, ("wgc", wi))
        load_w(wao_t[wi], w_ao_v, mt * 128, 128, ("wao", wi))
        load_w(wga_t[wi], w_in_v, C_GA + mt * 128, 128, ("wga", wi))
        for j in range(4):
            i = j % 2
            b = i * 4
            to = j * 512
            for ft in range(8):
                mm(psf(b), wco_t[wi][:, ft, :], uT[:, ft, to:to + 512], ft == 0, ft == 7, r=[("wco", wi), "uT"], w=[("ps", b)])
            proj_fm(psf(b + 1), ("ps", b + 1), wgc_t[wi], ("wgc", wi), xT_all, T + to, 512)
            for p_ in range(4):
                mm(psf(b + 2), wao_t[wi][:, p_, :], oT[:, p_, to:to + 512], p_ == 0, p_ == 3, r=[("wao", wi), "oT"], w=[("ps", b + 2)])
            proj_fm(psf(b + 3), ("ps", b + 3), wga_t[wi], ("wga", wi), xT_all, T + to, 512)
            act(sg[i], psf(b + 1), AF.Sigmoid, r=[("ps", b + 1), "gbT"], w=[("sg", i)], bias=gbT[:, 0, mt, :])
            act(sga[i], psf(b + 3), AF.Sigmoid, r=[("ps", b + 3), "gbT"], w=[("sga", i)], bias=gbT[:, 1, mt, :])
            tt("dve", m1[i], sg[i], psf(b), ALU.mult, r=[("sg", i), ("ps", b)], w=[("m1", i)])
            tt("dve", m2[i], sga[i], psf(b + 2), ALU.mult, r=[("sga", i), ("ps", b + 2)], w=[("m2", i)])
            tt("pool", mergedT[:, mt, to:to + 512], m1[i], m2[i], ALU.add, r=[("m1", i), ("m2", i)], w=["mergedT"])
    S.barrier()

    h = A.view(8 * KB, [NT, D], F32)
    wout = A.view(104 * KB, [8, D], BF16)
    load_w(wout[:, :, 0:512], w_out_v, 0, 512, "wout")
    load_w(wout[:, :, 512:1024], w_out_v, 512, 512, "wout")
    for tau in range(NT):
        dma("sp", h[:, tau, :], x_own[tau * 128:(tau + 1) * 128, :], w=[("h", tau)])
        for half in range(2):
            b = (2 * tau + half) % 4
            for c in range(8):
                mm(psf(b), mergedT[:, c, tau * 128:(tau + 1) * 128], wout[:, c, half * 512:(half + 1) * 512],
                   c == 0, c == 7, r=["mergedT", "wout"], w=[("ps", b)])
            tt("dve", h[:, tau, half * 512:(half + 1) * 512], psf(b), h[:, tau, half * 512:(half + 1) * 512], ALU.add,
               r=[("ps", b), ("h", tau)], w=[("h", tau)])
    S.barrier()

    finals = []
    if stage == "mixer":
        for tau in range(NT):
            finals.append(dma("sp", out[tau * 128:(tau + 1) * 128, :], h[:, tau, :], r=[("h", tau)]))
        S.emit("sp", finals)
        return nc, S

    build_peer(nc, S, A, ps, psf, psb, h, locals())
    return nc, S


def build_peer(nc, S, A, ps, psf, psb, h, L):
    mm, tr, act, tt, ts, stt, cp, dma = (L[n] for n in ("mm", "tr", "act", "tt", "ts", "stt", "cp", "dma"))
    cast_load, proj_fm, norm_phase = L["cast_load"], L["proj_fm"], L["norm_phase"]
    WST, wst_n = L["WST"], L["wst_n"]
    ident_f, ident_b, gffnT = L["ident_f"], L["ident_b"], L["gffnT"]
    wqry_v, skeys, pdown, pup, n_fin, out, stage = (L[n] for n in ("wqry_v", "skeys", "pdown", "pup", "n_fin", "out", "stage"))
    NEG = -1.0e30
    NBLK = 128

    dnT_d = nc.dram_tensor("dnT_scr", [NBLK, 128, 8, 128], BF16, kind="Internal").ap()
    up_d = nc.dram_tensor("up_scr", [NBLK * 128, D], BF16, kind="Internal").ap()
    wq_d = nc.dram_tensor("wq_scr", [16, 128, 8, 128], BF16, kind="Internal").ap()

    hnT = A.view(72 * KB, [8, T], BF16)
    PBm = Bump(A, 104 * KB, 190 * KB)
    skT = PBm.alloc([16, 128], BF16)
    lo = PBm.top

    NB_ = Bump(A, lo, 190 * KB)
    norm_phase([("sbuf", h[:, i, :], ("h", i)) for i in range(NT)], gffnT, hnT, "hnT", NB_)
    SB_ = Bump(A, NB_.top, 190 * KB)
    sk_b = [SB_.alloc([128], BF16) for _ in range(2)]
    for j in range(16):
        i = wst_n[0] % 4
        wst_n[0] += 1
        st = WST[i][:, 0, :]
        dma("sp", st, skeys[j, :, :], w=[("wst", i)])
        cp("pool", sk_b[j % 2], st, r=[("wst", i)], w=[("sk_b", j % 2)])
        b = 4 + j % 2
        tr(psb(b)[:, 0:128], sk_b[j % 2], ident_b, r=[("sk_b", j % 2), "ident_b"], w=[("ps", b)])
        act(skT[:, j, :], psb(b)[:, 0:128], AF.Copy, r=[("ps", b)], w=["skT"])
    S.barrier()

    XB = Bump(A, lo, 190 * KB)
    Dn_b = [XB.alloc([D], BF16) for _ in range(2)]
    Up_b = [XB.alloc([D], BF16) for _ in range(2)]
    DnT_o = [XB.alloc([8, 128], BF16) for _ in range(2)]
    stg = {}

    def pre_load(blk):
        i = wst_n[0] % 4
        wst_n[0] += 1
        i2 = wst_n[0] % 4
        wst_n[0] += 1
        dma("sp", WST[i].rearrange("p a b -> p (a b)"), pdown[blk * 128:(blk + 1) * 128, :], w=[("wst", i)])
        dma("sp", WST[i2].rearrange("p a b -> p (a b)"), pup[blk * 128:(blk + 1) * 128, :], w=[("wst", i2)])
        stg[blk] = (i, i2)

    wq_o = [XB.alloc([8, 128], BF16) for _ in range(2)]
    for j in range(16):
        i = wst_n[0] % 4
        wst_n[0] += 1
        dma("sp", WST[i], wqry_v[:, :, j * 128:(j + 1) * 128], w=[("wst", i)])
        cp("dve", wq_o[j % 2], WST[i], r=[("wst", i)], w=[("wq_o", j % 2)])
        dma("sp", wq_d[j], wq_o[j % 2], r=[("wq_o", j % 2)], w=[("wq_d", j)])
    pre_load(0)
    for blk in range(NBLK):
        if blk + 1 < NBLK:
            pre_load(blk + 1)
        i, i2 = stg.pop(blk)
        k2 = blk % 2
        act(Dn_b[k2], WST[i].rearrange("p a b -> p (a b)"), AF.Copy, r=[("wst", i)], w=[("Dn_b", k2)])
        cp("dve", Up_b[k2], WST[i2].rearrange("p a b -> p (a b)"), r=[("wst", i2)], w=[("Up_b", k2)])
        b = 4 + blk % 4
        for c in range(8):
            tr(psb(b)[:, c * 128:(c + 1) * 128], Dn_b[k2][:, c * 128:(c + 1) * 128], ident_b,
               r=[("Dn_b", k2), "ident_b"], w=[("ps", b)])
        cp("dve", DnT_o[k2], psb(b).rearrange("p (c e) -> p c e", c=8), r=[("ps", b)], w=[("DnT_o", k2)])
        dma("sp", dnT_d[blk], DnT_o[k2], r=[("DnT_o", k2)], w=[("dnT_d", blk)])
        dma("sp", up_d[blk * 128:(blk + 1) * 128, :], Up_b[k2], r=[("Up_b", k2)], w=[("up_d", blk)])
    S.barrier()

    GB = Bump(A, lo, 190 * KB)
    s_sb = GB.alloc([2, 16, 128], F32)
    E2 = GB.alloc([2, 8, 128], F32)
    E1q = GB.alloc([2, 8, 128], F32)
    Dg = GB.alloc([2, 8, 128], BF16)
    lo2 = GB.top
    s5 = s_sb.rearrange("p t (h c) k -> p t h c k", c=2)

    for G in range(NT // 2):
        t0 = G * 256
        SU = Bump(A, lo2, 190 * KB)
        qpT = SU.alloc([16, 256], BF16)
        wq_t = [SU.alloc([8, 128], BF16) for _ in range(2)]
        TT = []
        for _t in range(2):
            TT.append(dict(cand=SU.alloc([8, 256], F32), vals=SU.alloc([256], F32),
                           work=[SU.alloc([256], F32) for _ in range(2)], best=SU.alloc([8, 16], F32),
                           m3=SU.alloc([8, 8], F32), thr=SU.alloc([8, 1], F32), dd=SU.alloc([8, 16], F32),
                           Z=SU.alloc([8, 1], F32), rZ=SU.alloc([8, 1], F32), gthr=SU.alloc([8, 1], F32)))
        for j in range(16):
            wi = j % 2
            dma("sp", wq_t[wi], wq_d[j], r=[("wq_d", j)], w=[("wqp", wi)])
            b = 4 + j % 2
            proj_fm(psf(b, 0, 256), ("ps", b), wq_t[wi], ("wqp", wi), hnT, t0, 256)
            act(qpT[:, j, :], psf(b, 0, 256), AF.Copy, r=[("ps", b)], w=["qpT"])
        def tile_gen(tt_):
            cand, vals, work, best, m3, thr, dd, Z, rZ, gthr = (TT[tt_][n] for n in
                ("cand", "vals", "work", "best", "m3", "thr", "dd", "Z", "rZ", "gthr"))
            K_ = lambda n: (n, tt_)
            for jj in range(4):
                b = 6 + jj % 2
                for j4 in range(4):
                    j = jj * 4 + j4
                    mm(psf(b, j4 * 128, 128), qpT[:, j, tt_ * 128:(tt_ + 1) * 128], skT[:, j, :], True, True,
                       r=["qpT", "skT"], w=[("ps", b)])
                cp("dve", s_sb[:, tt_, jj * 4:(jj + 1) * 4, :], psf(b).rearrange("p (j k) -> p j k", j=4),
                   r=[("ps", b)], w=[("s_sb", tt_, jj)])
                yield
            skeys_ = [("s_sb", tt_, jj) for jj in range(4)]
            v_a = vals.rearrange("p (h c a o) -> p h c a o", h=8, c=2, a=16, o=1)
            v_b = vals.rearrange("p (h c o a) -> p h c o a", h=8, c=2, a=16, o=1)
            v3 = vals.rearrange("p (j a) -> p j a", j=16)
            for j in range(16):
                src = s_sb[:, tt_, j, :]
                sk = [("s_sb", tt_, j // 4)]
                S.op("dve", lambda e, j=j, src=src: e.max(out=v3[:, j, 0:8], in_=src), r=sk, w=[K_("vals")])
                yield
                S.op("dve", lambda e, j=j, src=src: e.match_replace(out=work[0][:, 0:128], in_to_replace=v3[:, j, 0:8],
                                                                    in_values=src, imm_value=NEG),
                     r=sk + [K_("vals")], w=[K_("work0")])
                yield
                S.op("dve", lambda e, j=j: e.max(out=v3[:, j, 8:16], in_=work[0][:, 0:128]), r=[K_("work0")], w=[K_("vals")])
                yield
            for hd in range(8):
                tt("dve", cand[:, hd, :].rearrange("p (a b) -> p a b", a=16),
                   v_a[:, hd, 0, :, 0:1].to_broadcast([128, 16, 16]),
                   v_b[:, hd, 1, 0:1, :].to_broadcast([128, 16, 16]), ALU.add, r=[K_("vals")], w=[K_("cand")])
                yield
            for hd in range(8):
                ch = cand[:, hd, :]
                S.op("dve", lambda e, hd=hd, ch=ch: e.max(out=best[:, hd, 0:8], in_=ch), r=[K_("cand")], w=[K_("best")])
                yield
                S.op("dve", lambda e, hd=hd, ch=ch: e.match_replace(out=work[0], in_to_replace=best[:, hd, 0:8],
                                                                    in_values=ch, imm_value=NEG),
                     r=[K_("cand"), K_("best")], w=[K_("work0")])
                yield
                S.op("dve", lambda e, hd=hd: e.max(out=best[:, hd, 8:16], in_=work[0]), r=[K_("work0")], w=[K_("best")])
                yield
                S.op("dve", lambda e, hd=hd: e.match_replace(out=work[1], in_to_replace=best[:, hd, 8:16],
                                                             in_values=work[0], imm_value=NEG),
                     r=[K_("work0"), K_("best")], w=[K_("work1")])
                yield
                S.op("dve", lambda e, hd=hd: e.max(out=m3[:, hd, :], in_=work[1]), r=[K_("work1")], w=[K_("m3")])
                yield
            tt("dve", thr, best[:, :, 15:16], m3[:, :, 0:1], ALU.add, r=[K_("best"), K_("m3")], w=[K_("thr")])
            yield
            ts("dve", thr, thr, 0.5, None, ALU.mult, r=[K_("thr")], w=[K_("thr")])
            yield
            tt("dve", dd, best, best[:, :, 0:1].to_broadcast([128, 8, 16]), ALU.subtract, r=[K_("best")], w=[K_("dd")])
            yield
            act(dd, dd, AF.Exp, r=[K_("dd")], w=[K_("dd")])
            S.op("dve", lambda e: e.tensor_reduce(out=Z, in_=dd, axis=AX.X, op=ALU.add), r=[K_("dd")], w=[K_("Z")])
            yield
            S.op("dve", lambda e: e.reciprocal(out=rZ, in_=Z), r=[K_("Z")], w=[K_("rZ")])
            yield
            s1v = s5[:, tt_, :, 0, :]
            s2v = s5[:, tt_, :, 1, :]
            tt("dve", gthr, thr, best[:, :, 0:1], ALU.subtract, r=[K_("thr"), K_("best")], w=[K_("gthr")])
            yield
            act(gthr, gthr, AF.Exp, r=[K_("gthr")], w=[K_("gthr")])
            tt("dve", gthr, gthr, rZ, ALU.mult, r=[K_("gthr"), K_("rZ")], w=[K_("gthr")])
            yield
            tt("dve", E1q[:, tt_, :, :], s1v, thr[:, :, 0:1].to_broadcast([128, 8, 128]), ALU.subtract,
               r=skeys_ + [K_("thr")], w=[K_("E1q")])
            yield
            act(E1q[:, tt_, :, :], E1q[:, tt_, :, :], AF.Exp, r=[K_("E1q")], w=[K_("E1q")])
            act(E2[:, tt_, :, :], s2v, AF.Exp, r=skeys_, w=[K_("E2")])
            for hd in range(8):
                ts("dve", Dg[:, tt_, hd, :], ident_f, gthr[:, hd, :], None, ALU.mult, r=["ident_f", K_("gthr")], w=[("Dg", tt_, hd)])
                yield

        gens = [tile_gen(0), tile_gen(1)]
        alive = [True, True]
        while any(alive):
            for gi in range(2):
                if alive[gi]:
                    try:
                        next(gens[gi])
                    except StopIteration:
                        alive[gi] = False
        S.barrier()

        LB = Bump(A, lo2, 190 * KB)
        DnT = [LB.alloc([8, 128], BF16) for _ in range(3)]
        Upb = [LB.alloc([D], BF16) for _ in range(4)]
        GA = [LB.alloc([256], BF16) for _ in range(3)]
        mt_ = [LB.alloc([2, 8, 128], F32) for _ in range(2)]
        mb = [LB.alloc([2, 8, 128], BF16) for _ in range(2)]
        WA = [LB.alloc([256], BF16) for _ in range(2)]
        E2f = E2.rearrange("p t h k -> p (t h) k")
        E1qf = E1q.rearrange("p t h k -> p (t h) k")

        def loads(i):
            dma("sp", DnT[i % 3], dnT_d[i], r=[("dnT_d", i)], w=[("DnT", i % 3)])
            dma("sp", Upb[i % 4], up_d[i * 128:(i + 1) * 128, :], r=[("up_d", i)], w=[("Upb", i % 4)])

        loads(0)
        N = NBLK if CUT[0] != 77 else 2
        for it in range(N + 2):
            if it + 1 < N:
                loads(it + 1)
            if it < N:
                i = it
                ND = 9
                mt_f = mt_[i % 2].rearrange("p t h k -> p (t h) k")
                tt("dve", mt_f[:, 0:ND, :], E2f[:, 0:ND, :], E1qf[:, 0:ND, i:i + 1].to_broadcast([128, ND, 128]), ALU.mult,
                   r=["E2", "E1q"], w=[("mt", i % 2)])
                for ch in range(ND, 16):
                    act(mt_f[:, ch, :], E2f[:, ch, :], AF.Copy, r=["E2", "E1q"], w=[("mt1", i % 2, ch)],
                        scale=E1qf[:, ch, i:i + 1])
                b = 4 + i % 2
                for c in range(8):
                    mm(psf(b, 0, 256), DnT[i % 3][:, c, :], hnT[:, c, t0:t0 + 256], c == 0, c == 7,
                       r=[("DnT", i % 3)], w=[("ps", b)])
                act(GA[i % 3], psf(b, 0, 256), AF.Gelu, r=[("ps", b)], w=[("GA", i % 3)])
            if 1 <= it <= N:
                i = it - 1
                stt(mb[i % 2], mt_[i % 2], 1.0, mt_[i % 2], ALU.is_ge, ALU.mult, r=[("mt", i % 2)] + [("mt1", i % 2, ch) for ch in range(9, 16)], w=[("mb", i % 2)])
                b = 6 + i % 2
                for tt_ in range(2):
                    for hd in (range(8) if CUT[0] != 78 else [0, 7]):
                        mm(psf(b, tt_ * 128, 128), mb[i % 2][:, tt_, hd, :], Dg[:, tt_, hd, :], hd == 0, hd == 7,
                           r=[("mb", i % 2), "Dg"], w=[("ps", b)])
            if 2 <= it <= N + 1:
                i = it - 2
                b = 6 + i % 2
                tt("dve", WA[i % 2], psf(b, 0, 256), GA[i % 3], ALU.mult, r=[("ps", b), ("GA", i % 3)], w=[("WA", i % 2)])
                for tt_ in range(2):
                    for half in range(2):
                        bo = tt_ * 2 + half
                        mm(psf(bo), WA[i % 2][:, tt_ * 128:(tt_ + 1) * 128], Upb[i % 4][:, half * 512:(half + 1) * 512],
                           i == 0, i == N - 1, r=[("WA", i % 2), ("Upb", i % 4)], w=[("ps", bo)])
        for tt_ in range(2):
            for half in range(2):
                bo = tt_ * 2 + half
                tau = 2 * G + tt_
                tt("dve", h[:, tau, half * 512:(half + 1) * 512], psf(bo), h[:, tau, half * 512:(half + 1) * 512], ALU.add,
                   r=[("ps", bo), ("h", tau)], w=[("h", tau)])
        S.barrier()

    finals = []
    if stage == "full_h2":
        for tau in range(NT):
            finals.append(dma("sp", out[tau * 128:(tau + 1) * 128, :], h[:, tau, :], r=[("h", tau)]))
        S.emit("sp", finals)
        return

    FB = Bump(A, 72 * KB, 190 * KB)
    fin_bc = FB.alloc([D], F32)
    junk = FB.alloc([D], BF16)
    ob = [FB.alloc([D], F32) for _ in range(2)]
    ss = [FB.alloc([1], F32) for _ in range(2)]
    sd = [FB.alloc([1], F32) for _ in range(2)]
    rstd = [FB.alloc([1], F32) for _ in range(2)]
    dma("sp", fin_bc, n_fin.partition_broadcast(128), w=["fin_bc"])
    for tau in range(NT):
        k = tau % 2
        act(junk, h[:, tau, :], AF.Square, r=[("h", tau)], w=["fjunk", ("fss", k)], accum_out=ss[k])
        act(sd[k], ss[k], AF.Sqrt, r=[("fss", k)], w=[("fsd", k)], scale=1.0 / D, bias=1e-6)
        S.op("dve", lambda e, k=k: e.reciprocal(out=rstd[k], in_=sd[k]), r=[("fsd", k)], w=[("frstd", k)])
        stt(ob[k], h[:, tau, :], rstd[k], fin_bc, ALU.mult, ALU.mult, r=[("h", tau), ("frstd", k), "fin_bc"], w=[("ob", k)])
        finals.append(dma("sp", out[tau * 128:(tau + 1) * 128, :], ob[k], r=[("ob", k)]))
    S.emit("sp", finals)


def make_consts():
    c = {}
    c["c_ident"] = np.eye(128, dtype=np.float32)
    k = np.arange(128)[:, None]
    q = np.arange(128)[None, :]
    c["c_mask"] = np.concatenate([(k >= q), (k <= q)], axis=1).astype(np.float32)
    e1 = np.zeros((128, 516), np.float32)
    e1[np.arange(128), 4 + 4 * np.arange(128)] = 1.0
    e2 = np.zeros((128, 2064), np.float32)
    e2[np.arange(128), 16 + 16 * np.arange(128)] = 1.0
    c["c_e1"], c["c_e2"] = e1, e2
    inv_freq = (np.float32(500000.0) ** (-np.arange(8, dtype=np.float32) * np.float32(2.0 / 16))).astype(np.float32)
    rope = np.zeros((128, 2), np.float32)
    for p in range(128):
        d = p % 64
        if d < 16:
            rope[p, 0] = inv_freq[d % 8]
            rope[p, 1] = -1.0 if d < 8 else 1.0
    c["c_rope"] = rope
    return c


def make_in_maps(inputs, stage="full"):
    x = np.asarray(inputs["x"], np.float32)
    positions = np.asarray(inputs["positions"], np.int32)
    consts = make_consts()
    shared = {
        "w_in": np.ascontiguousarray(inputs["w_in"][0]),
        "conv_w": np.ascontiguousarray(inputs["conv_w"][0]),
        "w_conv_out": np.ascontiguousarray(inputs["w_conv_out"][0]),
        "w_attn_out": np.ascontiguousarray(inputs["w_attn_out"][0]),
        "gate_bias": np.ascontiguousarray(inputs["gate_bias"][0]),
        "w_out": np.ascontiguousarray(inputs["w_out"][0]),
        "norm_mix": np.ascontiguousarray(inputs["norm_mix"][0]),
        "norm_ffn": np.ascontiguousarray(inputs["norm_ffn"][0]),
        "final_norm": np.ascontiguousarray(inputs["final_norm"]),
        "peer_w_query": np.ascontiguousarray(inputs["peer_w_query"][0]),
        "peer_sub_keys": np.ascontiguousarray(inputs["peer_sub_keys"][0]).reshape(16, 128, 128),
        "peer_down": np.ascontiguousarray(inputs["peer_down"][0]),
        "peer_up": np.ascontiguousarray(inputs["peer_up"][0]),
    }
    shared = {k: np.asarray(v, np.float32) for k, v in shared.items()}
    shared.update(consts)
    if stage not in ("full", "full_h2"):
        shared.pop("peer_down"); shared.pop("peer_up")
    maps = []
    for core in range(8):
        b, q = core // 4, core % 4
        m = dict(shared)
        m["x_own"] = np.ascontiguousarray(x[b, q * T:(q + 1) * T])
        if q > 0:
            m["x_halo"] = np.ascontiguousarray(x[b, (q - 1) * T:q * T])
            ph = positions[b, (q - 1) * T:q * T]
        else:
            m["x_halo"] = np.zeros((T, D), np.float32)
            ph = np.zeros((T,), np.int32)
        m["pos"] = np.ascontiguousarray(np.concatenate([ph, positions[b, q * T:(q + 1) * T]])[None, :]).astype(np.int32)
        m["halo_flag"] = np.full((128, 1), 1.0 if q > 0 else 0.0, np.float32)
        maps.append(m)
    return maps


_CACHE = {}


def kernel(**inputs):
    stage = inputs.pop("_stage", "full")
    if stage not in _CACHE:
        _CACHE[stage] = build(stage)
    nc, _ = _CACHE[stage]
    maps = make_in_maps(inputs, stage)
    res = run_bass_kernel_spmd(nc, maps, core_ids=list(range(8)))
    outs = [np.asarray(r["out"], np.float32) for r in res.results]
    full = np.stack([np.concatenate(outs[0:4], axis=0), np.concatenate(outs[4:8], axis=0)], axis=0)
    return full.astype(np.float32)
```

```python
import numpy as np
import concourse.bass as bass
import concourse.mybir as mybir
from concourse.bass_utils import run_bass_kernel_spmd

F32 = mybir.dt.float32
BF16 = mybir.dt.bfloat16
I32 = mybir.dt.int32
U8 = mybir.dt.uint8
AF = mybir.ActivationFunctionType
ALU = mybir.AluOpType
AX = mybir.AxisListType

ENGS = ["pe", "act", "dve", "pool", "sp"]
NDMASEM = 8


class Sched:
    def __init__(self, nc):
        self.nc = nc
        self.ops = {e: [] for e in ENGS}
        self.last_w = {}
        self.readers = {}
        self.dma_count = {"sp": 0, "pool": 0, "act": 0}
        self.same_engine_sync = {"pe": False, "act": True, "dve": True, "pool": True, "sp": False}
        self.global_deps = []
        self.open_dmas = []

    def barrier(self):
        toks = []
        for e in ENGS:
            for i in range(len(self.ops[e]) - 1, -1, -1):
                if self.ops[e][i]["dma"] is None:
                    toks.append(("e", e, i))
                    break
        toks.extend(self.open_dmas)
        self.open_dmas = []
        self.global_deps = toks

    def _deps(self, reads, writes):
        deps = []
        for r in reads:
            t = self.last_w.get(r)
            if t is not None:
                deps.append(t)
        for w in writes:
            t = self.last_w.get(w)
            if t is not None:
                deps.append(t)
            deps.extend(self.readers.get(w, ()))
        return deps

    def _commit(self, tok, reads, writes):
        for r in reads:
            self.readers.setdefault(r, []).append(tok)
        for w in writes:
            self.last_w[w] = tok
            self.readers[w] = []

    @staticmethod
    def _excl(r, w):
        r2 = [k for k in r if not (isinstance(k, tuple) and k[0] == "ps")]
        w2 = list(w) + [k for k in r if isinstance(k, tuple) and k[0] == "ps"]
        return r2, w2

    def op(self, eng, fn, r=(), w=()):
        r, w = self._excl(r, w)
        deps = self._deps(r, w)
        idx = len(self.ops[eng])
        waits = []
        for t in list(deps) + self.global_deps:
            if t[0] == "e" and t[1] == eng and not self.same_engine_sync[eng]:
                continue
            waits.append(t)
        self.ops[eng].append(dict(fn=fn, waits=waits, dma=None))
        tok = ("e", eng, idx)
        self._commit(tok, r, w)
        return tok

    def dma(self, q, fn, r=(), w=()):
        r, w = self._excl(r, w)
        deps = self._deps(r, w)
        n = self.dma_count[q]
        self.dma_count[q] += 1
        slot, gen = n % NDMASEM, n // NDMASEM
        waits = list(deps) + [t for t in self.global_deps if not (t[0] == "e" and t[1] == q and not self.same_engine_sync[q])]
        if gen > 0:
            waits.append(("d", q, slot, gen))
        self.ops[q].append(dict(fn=fn, waits=waits, dma=(q, slot)))
        tok = ("d", q, slot, gen + 1)
        self.open_dmas.append(tok)
        self._commit(tok, r, w)
        return tok

    def emit(self, final_waits_engine="sp", final_toks=()):
        nc = self.nc
        needed = {e: set() for e in ENGS}
        for e in ENGS:
            for o in self.ops[e]:
                for t in o["waits"]:
                    if t[0] == "e":
                        needed[t[1]].add(t[2])
        for t in final_toks:
            if t[0] == "e":
                needed[t[1]].add(t[2])
        cnt = {}
        for e in ENGS:
            c = 0
            m = {}
            for i in range(len(self.ops[e])):
                if i in needed[e]:
                    c += 1
                    m[i] = c
            cnt[e] = m
        self.max_sem = {e: (max(cnt[e].values()) if cnt[e] else 0) for e in ENGS}
        esem = {e: nc.alloc_semaphore(name=f"prog_{e}") for e in ENGS}
        dsem = {q: [nc.alloc_semaphore(name=f"dma_{q}_{i}") for i in range(NDMASEM)]
                for q in ("sp", "pool", "act")}
        handles = {"pe": "tensor", "act": "scalar", "dve": "vector", "pool": "gpsimd", "sp": "sync"}

        def tokval(t):
            if t[0] == "e":
                return esem[t[1]], cnt[t[1]][t[2]]
            return dsem[t[1]][t[2]], 16 * t[3]

        def run_engine(ename, eng):
            waited = {}
            for i, o in enumerate(self.ops[ename]):
                for t in o["waits"]:
                    s, v = tokval(t)
                    key = id(s)
                    if waited.get(key, 0) >= v:
                        continue
                    eng.wait_ge(s, v)
                    waited[key] = v
                inst = o["fn"](eng)
                if o["dma"] is not None:
                    q, slot = o["dma"]
                    inst.then_inc(dsem[q][slot], 16)
                elif i in needed[ename]:
                    inst.then_inc(esem[ename], 1)
            if ename == final_waits_engine:
                for t in final_toks:
                    s, v = tokval(t)
                    eng.wait_ge(s, v)

        with nc.Block() as block:
            for ename in ENGS:
                deco = getattr(block, handles[ename])

                def body(eng, ename=ename):
                    run_engine(ename, eng)
                deco(body)


class Arena:
    def __init__(self, nc, nbytes, name="arena"):
        self.t = nc.alloc_sbuf_tensor(name, [128, nbytes], U8)
        self.n = nbytes
        self.top = 0

    def mark(self):
        return self.top

    def release(self, m):
        self.top = m

    def alloc(self, shape_free, dtype, parts=128):
        esz = {F32: 4, BF16: 2, I32: 4}[dtype]
        n = int(np.prod(shape_free)) * esz
        off = (self.top + 63) // 64 * 64
        assert off + n <= self.n, f"arena overflow {off}+{n}>{self.n}"
        self.top = off + n
        ap = self.t[0:parts, off:off + n].bitcast(dtype)
        if len(shape_free) > 1:
            names = " ".join(f"d{i}" for i in range(len(shape_free)))
            kw = {f"d{i}": int(s) for i, s in enumerate(shape_free)}
            ap = ap.rearrange(f"p ({names}) -> p {names}", **kw)
        return ap

    def view(self, off, shape_free, dtype, parts=128):
        esz = {F32: 4, BF16: 2, I32: 4}[dtype]
        n = int(np.prod(shape_free)) * esz
        assert off % 4 == 0 and off + n <= self.n, f"arena overflow {off}+{n}>{self.n}"
        ap = self.t[0:parts, off:off + n].bitcast(dtype)
        if len(shape_free) > 1:
            names = " ".join(f"d{i}" for i in range(len(shape_free)))
            kw = {f"d{i}": int(s) for i, s in enumerate(shape_free)}
            ap = ap.rearrange(f"p ({names}) -> p {names}", **kw)
        return ap


class Bump:
    def __init__(self, arena, lo, hi):
        self.a, self.lo, self.hi, self.top = arena, lo, hi, lo

    def alloc(self, shape_free, dtype, parts=128):
        esz = {F32: 4, BF16: 2, I32: 4}[dtype]
        n = int(np.prod(shape_free)) * esz
        off = (self.top + 63) // 64 * 64
        assert off + n <= self.hi, f"bump overflow {off}+{n}>{self.hi}"
        self.top = off + n
        return self.a.view(off, shape_free, dtype, parts)


KB = 1024
CUT = [0]
VBANK = [7]
D = 1024
T = 2048
NT = 16
C_B, C_C, C_X, C_Q, C_K, C_V, C_GC, C_GA = 0, 1024, 2048, 3072, 4608, 6144, 7680, 8704
GROUPS = [(1, 128), (4, 512), (16, 2048)]
MAGIC = 12582912.0
TWO_PI = 6.283185307179586
CW1 = 6.28125
CW2 = TWO_PI - CW1
PI_S = 3.1415925


def build(stage="full"):
    nc = bass.Bass("TRN2", target_bir_lowering=False)

    def din(name, shape, dt=F32):
        return nc.dram_tensor(name, list(shape), dt, kind="ExternalInput").ap()

    x_own = din("x_own", [T, D])
    x_halo = din("x_halo", [T, D])
    pos = din("pos", [1, 2 * T], I32)
    flag_d = din("halo_flag", [128, 1])
    w_in = din("w_in", [D, 9728])
    conv_w = din("conv_w", [3, D])
    w_co = din("w_conv_out", [D, D])
    w_ao = din("w_attn_out", [512, D])
    gbias = din("gate_bias", [2, D])
    w_out = din("w_out", [D, D])
    n_mix = din("norm_mix", [D])
    n_ffn = din("norm_ffn", [D])
    n_fin = din("final_norm", [D])
    wqry = din("peer_w_query", [D, 2048])
    skeys = din("peer_sub_keys", [16, 128, 128])
    if stage in ("full", "full_h2"):
        pdown = din("peer_down", [16384, D])
        pup = din("peer_up", [16384, D])
    c_ident = din("c_ident", [128, 128])
    c_mask = din("c_mask", [128, 256])
    c_e1 = din("c_e1", [128, 516])
    c_e2 = din("c_e2", [128, 2064])
    c_rope = din("c_rope", [128, 2])
    out = nc.dram_tensor("out", [T, D], F32, kind="ExternalOutput").ap()

    S = Sched(nc)
    A = Arena(nc, 206 * KB)
    ps = [nc.alloc_psum_tensor(f"psb{i}", [128, 512], F32) for i in range(8)]

    def psf(b, c0=0, n=512):
        return ps[b][:, c0:c0 + n]

    def psb(b):
        return ps[b][:, :].bitcast(BF16)

    w_in_v = w_in.rearrange("(c p) n -> p c n", p=128)
    w_co_v = w_co.rearrange("(c p) n -> p c n", p=128)
    w_ao_v = w_ao.rearrange("(c p) n -> p c n", p=128)
    w_out_v = w_out.rearrange("(c p) n -> p c n", p=128)
    wqry_v = wqry.rearrange("(c p) n -> p c n", p=128)

    CB = Bump(A, 0, 8 * KB)
    ident_f = CB.alloc([128], F32)
    ident_b = CB.alloc([128], BF16)
    mask_b = CB.alloc([256], BF16)
    e1_b = CB.alloc([516], BF16)
    e2_b = CB.alloc([2064], BF16)
    rope = CB.alloc([2], F32)
    flag = CB.alloc([1], F32)
    gmixT = CB.alloc([8, 1], F32)
    gffnT = CB.alloc([8, 1], F32)
    convwT = CB.alloc([3, 8, 1], F32)
    gbT = CB.alloc([2, 8, 1], F32)

    def dma(q, out_ap, in_ap, r=(), w=(), slow=False):
        def fn(e, out_ap=out_ap, in_ap=in_ap):
            if slow:
                return e.dma_start(out=out_ap, in_=in_ap, allow_slow_non_contiguous=True)
            return e.dma_start(out=out_ap, in_=in_ap)
        return S.dma(q, fn, r=r, w=w)

    WST = [A.view(190 * KB + i * 4096, [8, 128], F32) for i in range(4)]
    wst_n = [0]

    def cast_load(dst, src, ncols_total, dkey, src_is_w=True):
        kc = dst.shape[1]
        for c0 in range(0, ncols_total, 128):
            n = min(128, ncols_total - c0)
            i = wst_n[0] % 4
            wst_n[0] += 1
            st = WST[i][:, 0:kc, 0:n]
            dma("sp", st, src[:, :, c0:c0 + n], w=[("wst", i)])
            S.op("pool", lambda e, st=st, d=dst[:, :, c0:c0 + n]: e.tensor_copy(out=d, in_=st),
                 r=[("wst", i)], w=[dkey])

    def cast_load2(dst, src, dkey):
        n = dst.shape[1]
        for c0 in range(0, n, 1024):
            m = min(1024, n - c0)
            i = wst_n[0] % 4
            wst_n[0] += 1
            st = WST[i].rearrange("p a b -> p (a b)")[:, 0:m]
            dma("sp", st, src[:, c0:c0 + m], w=[("wst", i)])
            S.op("pool", lambda e, st=st, d=dst[:, c0:c0 + m]: e.tensor_copy(out=d, in_=st),
                 r=[("wst", i)], w=[dkey])

    dma("sp", ident_f, c_ident, w=["ident_f"])
    cast_load2(ident_b, c_ident, "ident_b")
    cast_load2(mask_b, c_mask, "mask_b")
    cast_load2(e1_b, c_e1, "e1_b")
    cast_load2(e2_b, c_e2, "e2_b")
    dma("sp", rope, c_rope, w=["rope"])
    dma("sp", flag, flag_d, w=["flag"])
    dma("sp", gmixT, n_mix.rearrange("(c p o) -> p c o", p=128, o=1), w=["gmixT"], slow=True)
    dma("sp", gffnT, n_ffn.rearrange("(c p o) -> p c o", p=128, o=1), w=["gffnT"], slow=True)
    for i_ in range(3):
        dma("sp", convwT[:, i_, :, :], conv_w[i_, :].rearrange("(c p o) -> p c o", p=128, o=1), w=["convwT"], slow=True)
    for j_ in range(2):
        dma("sp", gbT[:, j_, :, :], gbias[j_, :].rearrange("(c p o) -> p c o", p=128, o=1), w=["gbT"], slow=True)

    def early_exit(off):
        S.barrier()
        src = A.view(off, [NT, D], F32)
        fin = [dma("sp", out.rearrange("(t p) d -> p t d", p=128), src)]
        S.emit("sp", fin)
        return nc, S

    if stage == "const":
        return early_exit(0)

    def mm(out_ap, lhsT, rhs, start, stop, r=(), w=()):
        return S.op("pe", lambda e: e.matmul(out_ap, lhsT=lhsT, rhs=rhs, start=start, stop=stop), r=r, w=w)

    def tr(out_ap, in_ap, ident, r=(), w=()):
        return S.op("pe", lambda e: e.transpose(out_ap, in_ap, ident), r=r, w=w)

    def act(out_ap, in_ap, func, r=(), w=(), **kw):
        return S.op("act", lambda e: e.activation(out=out_ap, in_=in_ap, func=func, **kw), r=r, w=w)

    def tt(eng, out_ap, in0, in1, op, r=(), w=()):
        return S.op(eng, lambda e: e.tensor_tensor(out=out_ap, in0=in0, in1=in1, op=op), r=r, w=w)

    def ts(eng, out_ap, in0, s1, s2, op0, op1=None, r=(), w=()):
        if op1 is None:
            return S.op(eng, lambda e: e.tensor_scalar(out=out_ap, in0=in0, scalar1=s1, scalar2=None, op0=op0), r=r, w=w)
        return S.op(eng, lambda e: e.tensor_scalar(out=out_ap, in0=in0, scalar1=s1, scalar2=s2, op0=op0, op1=op1), r=r, w=w)

    def stt(out_ap, in0, scalar, in1, op0, op1, r=(), w=()):
        return S.op("dve", lambda e: e.scalar_tensor_tensor(out=out_ap, in0=in0, scalar=scalar, in1=in1, op0=op0, op1=op1), r=r, w=w)

    def cp(eng, out_ap, in_ap, r=(), w=()):
        return S.op(eng, lambda e: e.tensor_copy(out=out_ap, in_=in_ap), r=r, w=w)

    def xkeys(t0, n, step=1):
        return [("xT", i) for i in range(t0 // 128, (t0 + (n - 1) * step) // 128 + 1)]

    def load_w(dst, src_v, c0, ncols, key):
        return cast_load(dst, src_v[:, :, c0:c0 + ncols], ncols, key)

    def proj_fm(ps_ap, pskey, wt, wkey, xT, t0, n, kc=8):
        for c in range(kc):
            mm(ps_ap, wt[:, c, :], xT[:, c, t0:t0 + n], c == 0, c == kc - 1,
               r=[wkey] + xkeys(t0, n), w=[pskey])

    xT_all = A.view(8 * KB, [8, 2 * T], BF16)
    cosT = A.view(72 * KB, [2 * T], F32)
    sinT = A.view(88 * KB, [2 * T], F32)
    oT = A.view(104 * KB, [4, T], BF16)

    TB = Bump(A, 104 * KB, 190 * KB)
    posi = TB.alloc([2 * T], I32)
    ang = TB.alloc([2 * T], F32)
    uu = TB.alloc([2 * T], F32)
    kf = TB.alloc([2 * T], F32)
    rr = TB.alloc([2 * T], F32)
    dma("sp", posi, pos[0, :].partition_broadcast(128), w=["posi"])
    cp("dve", ang, posi, r=["posi"], w=["ang"])
    ts("dve", ang, ang, rope[:, 0:1], None, ALU.mult, r=["ang", "rope"], w=["ang"])

    def sin_table(dst, dkey, shift, scale):
        ts("dve", uu, ang, float(shift), None, ALU.add, r=["ang"], w=["uu"])
        ts("dve", kf, uu, 1.0 / TWO_PI, MAGIC, ALU.mult, ALU.add, r=["uu"], w=["kf"])
        ts("dve", kf, kf, -MAGIC, None, ALU.add, r=["kf"], w=["kf"])
        stt(rr, kf, -CW1, uu, ALU.mult, ALU.add, r=["kf", "uu"], w=["rr"])
        stt(rr, kf, -CW2, rr, ALU.mult, ALU.add, r=["kf", "rr"], w=["rr"])
        ts("dve", rr, rr, -PI_S, PI_S, ALU.max, ALU.min, r=["rr"], w=["rr"])
        act(dst, rr, AF.Sin, r=["rr", "rope"], w=[dkey], scale=scale)

    sin_table(cosT, "cosT", np.pi / 2, 1.0)
    sin_table(sinT, "sinT", 0.0, rope[:, 1:2])
    S.barrier()
    if stage == "rope":
        return early_exit(72 * KB)

    def norm_phase(src_tiles, gT, dstT, dkey, B, tile0=0):
        xin = [B.alloc([D], F32) for _ in range(2)]
        xs = [B.alloc([D], BF16) for _ in range(2)]
        junk = B.alloc([D], BF16)
        ss = [B.alloc([1], F32) for _ in range(2)]
        sd = [B.alloc([1], F32) for _ in range(2)]
        rstd = [B.alloc([1], F32) for _ in range(2)]
        for i, src in enumerate(src_tiles):
            k = i % 2
            if src[0] == "dram":
                dma("sp", xin[k], src[1], w=[("xin", k)])
                xi, xkey = xin[k], ("xin", k)
            else:
                xi, xkey = src[1], src[2]
            if CUT[0] == 9:
                continue
            act(junk, xi, AF.Square, r=[xkey], w=["junk", ("ss", k)], accum_out=ss[k])
            if CUT[0] == 1:
                continue
            act(sd[k], ss[k], AF.Sqrt, r=[("ss", k)], w=[("sd", k)], scale=1.0 / D, bias=1e-6)
            if CUT[0] == 2:
                continue
            S.op("dve", lambda e, k=k: e.reciprocal(out=rstd[k], in_=sd[k]), r=[("sd", k)], w=[("rstd", k)])
            if CUT[0] == 3:
                continue
            act(xs[k], xi, AF.Copy, r=[xkey, ("rstd", k)], w=[("xs", k)], scale=rstd[k])
            if CUT[0] == 4:
                continue
            pb = psb(k)
            for c in range(8):
                tr(pb[:, c * 128:(c + 1) * 128], xs[k][:, c * 128:(c + 1) * 128], ident_b,
                   r=[("xs", k), "ident_b"], w=[("ps", k)])
            if CUT[0] == 5:
                continue
            tt("dve", dstT[:, :, (tile0 + i) * 128:(tile0 + i + 1) * 128],
               pb.rearrange("p (c t) -> p c t", c=8), gT[:, :, 0:1].to_broadcast([128, 8, 128]), ALU.mult,
               r=[("ps", k)], w=[(dkey, tile0 + i)])

    PB = Bump(A, 120 * KB, 190 * KB)
    tiles = [("dram", x_halo[i * 128:(i + 1) * 128, :]) for i in range(NT)] + \
            [("dram", x_own[i * 128:(i + 1) * 128, :]) for i in range(NT)]
    if stage.startswith("A") and len(stage) > 1:
        tiles = tiles[:int(stage[1:])]
    norm_phase(tiles, gmixT, xT_all, "xT", PB)
    S.barrier()
    if stage.startswith("A"):
        return early_exit(8 * KB)

    AB = Bump(A, 120 * KB, 190 * KB)
    wq_t = [AB.alloc([8, 128], BF16) for _ in range(2)]
    wqs_t = [AB.alloc([8, 128], BF16) for _ in range(2)]
    wk_t = [AB.alloc([8, 128], BF16) for _ in range(2)]
    wks_t = [AB.alloc([8, 128], BF16) for _ in range(2)]
    wv_t = [AB.alloc([8, 128], BF16) for _ in range(2)]
    Qbuf = AB.alloc([T], BF16)
    Kbuf = AB.alloc([2 * T], BF16)
    Vbuf = AB.alloc([32, 2, 66], BF16)
    U = [AB.alloc([16, 130], BF16) for _ in range(3)]
    tA = [AB.alloc([512], F32) for _ in range(2)]
    tB = [AB.alloc([512], F32) for _ in range(2)]
    Pt = [AB.alloc([256], BF16) for _ in range(4)]
    otok = [AB.alloc([2, 64], BF16) for _ in range(2)]
    rl = [AB.alloc([2, 1], F32) for _ in range(2)]
    for k in range(2):
        S.op("pool", lambda e, k=k: e.memset(wqs_t[k], 0.0), w=[("wqs", k)])
        S.op("pool", lambda e, k=k: e.memset(wks_t[k], 0.0), w=[("wks", k)])

    def build_sw(dst, dkey, src, skey):
        for hh in range(2):
            b = hh * 64
            cp("pool", dst[:, :, b:b + 8], src[:, :, b + 8:b + 16], r=[skey], w=[dkey])
            cp("pool", dst[:, :, b + 8:b + 16], src[:, :, b:b + 8], r=[skey], w=[dkey])

    def perm_out(buf, nblk, dil, span, lt0, cs):
        v4 = buf[:, 0:nblk * span].rearrange("p (n r i) -> p n i r", n=nblk, r=dil, i=128)
        if cs >= span:
            n0, nn = lt0 // span, cs // span
            return v4[:, n0:n0 + nn, :, :], (lambda a: a[:, 0:cs].rearrange("p (n i r) -> p n i r", n=nn, i=128, r=dil))
        n0 = lt0 // span
        i0, ni = (lt0 % span) // dil, cs // dil
        return v4[:, n0, i0:i0 + ni, :], (lambda a: a[:, 0:cs].rearrange("p (i r) -> p i r", r=dil))

    wcnt = [0]
    pcnt = [0]
    for pp in range(4):
        for g, (dil, span) in enumerate(GROUPS):
            hp = g * 4 + pp
            nb = T // span
            k0 = T - span
            nk = span + T
            wi = wcnt[0] % 2
            wcnt[0] += 1
            load_w(wq_t[wi], w_in_v, C_Q + hp * 128, 128, ("wq", wi))
            build_sw(wqs_t[wi], ("wqs", wi), wq_t[wi], ("wq", wi))
            load_w(wk_t[wi], w_in_v, C_K + hp * 128, 128, ("wk", wi))
            build_sw(wks_t[wi], ("wks", wi), wk_t[wi], ("wk", wi))
            load_w(wv_t[wi], w_in_v, C_V + hp * 128, 128, ("wv", wi))

            def rot_chunk(wt, wkey, wst, wskey, t0, cs, buf, bkey, nblk, lt0):
                i = pcnt[0] % 2
                pcnt[0] += 1
                b0 = i * 2
                proj_fm(psf(b0, 0, cs), ("ps", b0), wt, wkey, xT_all, t0, cs)
                proj_fm(psf(b0 + 1, 0, cs), ("ps", b0 + 1), wst, wskey, xT_all, t0, cs)
                tt("dve", tA[i][:, 0:cs], psf(b0, 0, cs), cosT[:, t0:t0 + cs], ALU.mult,
                   r=[("ps", b0), "cosT"], w=[("tA", i)])
                tt("dve", tB[i][:, 0:cs], psf(b0 + 1, 0, cs), sinT[:, t0:t0 + cs], ALU.mult,
                   r=[("ps", b0 + 1), "sinT"], w=[("tB", i)])
                ov, inr = perm_out(buf, nblk, dil, span, lt0, cs)
                tt("dve", ov, inr(tA[i]), inr(tB[i]), ALU.add, r=[("tA", i), ("tB", i)], w=[bkey])

            for j in range(4):
                rot_chunk(wq_t[wi], ("wq", wi), wqs_t[wi], ("wqs", wi), T + j * 512, 512, Qbuf, "Q", nb, j * 512)
            if stage == "att1":
                return early_exit(120 * KB)
            lt = 0
            while lt < nk:
                cs = min(512, nk - lt)
                rot_chunk(wk_t[wi], ("wk", wi), wks_t[wi], ("wks", wi), k0 + lt, cs, Kbuf, "K", nb + 1, lt)
                lt += cs
            if stage == "att2":
                return early_exit(120 * KB)
            nvb = (nb + 1) * dil
            S.op("dve", lambda e, nvb=nvb: e.memset(Vbuf[:, 0:nvb, :, 64:65], 1.0), w=["V"])
            ts("dve", Vbuf[:, 0:dil, :, 64:65], Vbuf[:, 0:dil, :, 64:65], flag[:, 0:1], None, ALU.mult,
               r=["flag"], w=["V"])
            if stage == "att3a":
                return early_exit(120 * KB)
            for blk in range(nvb):
                n_, r_ = blk // dil, blk % dil
                st = k0 + n_ * span + r_
                s4 = 6 + blk % 2
                pv = psf(s4, 0, 128)
                for c in range(8):
                    mm(pv, xT_all[:, c, st:st + 127 * dil + 1:dil], wv_t[wi][:, c, :], c == 0, c == 7,
                       r=[("wv", wi)] + xkeys(st, 128, dil), w=[("ps", s4)])
                if stage == "att3b":
                    continue
                for hh_ in range(2):
                    cp("dve", Vbuf[:, blk, hh_, 0:64], pv[:, hh_ * 64:(hh_ + 1) * 64],
                       r=[("ps", s4)], w=["V"])
            if stage in ("att3", "att3b"):
                return early_exit(120 * KB)
            Kb = Kbuf[:, 0:nk].rearrange("p (n r i) -> p n r i", n=nb + 1, r=dil, i=128)
            Qb = Qbuf[:, 0:T].rearrange("p (n r i) -> p n r i", n=nb, r=dil, i=128)
            Ug = U[g].rearrange("p b (h d) -> p b h d", h=2)
            items = [(b16, hh) for b16 in range(16) for hh in range(2)]

            def st_scores(ii, b16, hh):
                n_, r_ = b16 // dil, b16 % dil
                rows = slice(hh * 64, hh * 64 + 64)
                s2 = 4 + ii % 2
                pS = psf(s2, 0, 256)
                mm(pS[:, 0:128], Kb[rows, n_, r_, :], Qb[rows, n_, r_, :], True, True, r=["K", "Q"], w=[("ps", s2)])
                mm(pS[:, 128:256], Kb[rows, n_ + 1, r_, :], Qb[rows, n_, r_, :], True, True, r=["K", "Q"], w=[("ps", s2)])
                p4 = ii % 4
                act(Pt[p4], pS, AF.Exp, r=[("ps", s2)], w=[("Pt", p4)], scale=0.125)
                tt("dve", Pt[p4], Pt[p4], mask_b, ALU.mult, r=[("Pt", p4), "mask_b"], w=[("Pt", p4)])

            def st_pv(ii, b16, hh):
                n_, r_ = b16 // dil, b16 % dil
                p4 = ii % 4
                b6 = 6 + ii % 2
                pO = psf(b6, 0, 65)
                mm(pO, Pt[p4][:, 0:128], Vbuf[:, n_ * dil + r_, hh, 0:65], True, False, r=[("Pt", p4), "V"], w=[("ps", b6)])
                mm(pO, Pt[p4][:, 128:256], Vbuf[:, (n_ + 1) * dil + r_, hh, 0:65], False, True, r=[("Pt", p4), "V"], w=[("ps", b6)])
                cp("dve", Ug[:, b16, hh, :], pO, r=[("ps", b6)], w=[("U", g)])

            for ii in range(len(items) + 1):
                if ii < len(items):
                    st_scores(ii, *items[ii])
                if ii >= 1:
                    st_pv(ii - 1, *items[ii - 1])
            if stage == "att4":
                return early_exit(120 * KB)
        for tau in range(NT):
            c2 = tau % 2
            pC = psf(c2, 0, 130)
            ops = [(ident_b, U[0][:, tau, :], ("U", 0))]
            for r_ in range(4):
                off = 4 - r_ + (tau % 4) * 128
                ops.append((e1_b[:, off:off + 128], U[1][:, (tau // 4) * 4 + r_, :], ("U", 1)))
            for r_ in range(16):
                off = 16 - r_ + tau * 128
                ops.append((e2_b[:, off:off + 128], U[2][:, r_, :], ("U", 2)))
            for oi, (l_, r_ap, uk) in enumerate(ops):
                mm(pC, l_, r_ap, oi == 0, oi == len(ops) - 1, r=[uk, "ident_b", "e1_b", "e2_b"], w=[("ps", c2)])
            pC3 = pC.rearrange("p (h d) -> p h d", h=2)
            S.op("dve", lambda e, c2=c2, pC3=pC3: e.reciprocal(out=rl[c2], in_=pC3[:, :, 64:65]),
                 r=[("ps", c2)], w=[("rl", c2)])
            tt("dve", otok[c2], pC3[:, :, 0:64], rl[c2][:, :, 0:1].to_broadcast([128, 2, 64]), ALU.mult,
               r=[("ps", c2), ("rl", c2)], w=[("otok", c2)])
            pT = psb(2 + c2)[:, 0:128]
            tr(pT, otok[c2].rearrange("p h d -> p (h d)"), ident_b, r=[("otok", c2), "ident_b"], w=[("ps", 2 + c2)])
            act(oT[:, pp, tau * 128:(tau + 1) * 128], pT, AF.Copy, r=[("ps", 2 + c2)], w=["oT"])
    S.barrier()

    uT = A.view(120 * KB, [8, T], BF16)
    VB = Bump(A, 152 * KB, 190 * KB)
    zbuf = VB.alloc([T + 2], F32)
    acc = VB.alloc([T], F32)
    ctmp = [VB.alloc([512], F32) for _ in range(2)]
    wb_t = [VB.alloc([8, 128], BF16) for _ in range(2)]
    wc_t = [VB.alloc([8, 128], BF16) for _ in range(2)]
    wx_t = [VB.alloc([8, 128], BF16) for _ in range(2)]
    pc = [0]
    for ft in range(8):
        wi = ft % 2
        load_w(wc_t[wi], w_in_v, C_C + ft * 128, 128, ("wc", wi))
        load_w(wx_t[wi], w_in_v, C_X + ft * 128, 128, ("wx", wi))
        load_w(wb_t[wi], w_in_v, C_B + ft * 128, 128, ("wb", wi))
        chunks = [(T - 2, 2, 0)] + [(T + j * 512, 512, 2 + j * 512) for j in range(4)]
        for (t0, cs, z0) in chunks:
            i = pc[0] % 2
            pc[0] += 1
            b0 = i * 2
            proj_fm(psf(b0, 0, cs), ("ps", b0), wc_t[wi], ("wc", wi), xT_all, t0, cs)
            proj_fm(psf(b0 + 1, 0, cs), ("ps", b0 + 1), wx_t[wi], ("wx", wi), xT_all, t0, cs)
            act(ctmp[i][:, 0:cs], psf(b0, 0, cs), AF.Copy, r=[("ps", b0)], w=[("ctmp", i)])
            tt("dve", zbuf[:, z0:z0 + cs], ctmp[i][:, 0:cs], psf(b0 + 1, 0, cs), ALU.mult,
               r=[("ctmp", i), ("ps", b0 + 1)], w=["z"])
        ts("dve", acc, zbuf[:, 0:T], convwT[:, 0, ft, :], None, ALU.mult, r=["z", "convwT"], w=["acc"])
        stt(acc, zbuf[:, 1:T + 1], convwT[:, 1, ft, :], acc, ALU.mult, ALU.add, r=["z", "acc"], w=["acc"])
        stt(acc, zbuf[:, 2:T + 2], convwT[:, 2, ft, :], acc, ALU.mult, ALU.add, r=["z", "acc"], w=["acc"])
        for j in range(4):
            i = pc[0] % 2
            pc[0] += 1
            b0 = i * 2
            proj_fm(psf(b0), ("ps", b0), wb_t[wi], ("wb", wi), xT_all, T + j * 512, 512)
            tt("dve", uT[:, ft, j * 512:(j + 1) * 512], acc[:, j * 512:(j + 1) * 512], psf(b0), ALU.mult,
               r=["acc", ("ps", b0)], w=["uT"])
    S.barrier()

    mergedT = A.view(72 * KB, [8, T], BF16)
    MB = Bump(A, 152 * KB, 190 * KB)
    wco_t = [MB.alloc([8, 128], BF16) for _ in range(2)]
    wgc_t = [MB.alloc([8, 128], BF16) for _ in range(2)]
    wao_t = [MB.alloc([4, 128], BF16) for _ in range(2)]
    wga_t = [MB.alloc([8, 128], BF16) for _ in range(2)]
    sg = [MB.alloc([512], F32) for _ in range(2)]
    sga = [MB.alloc([512], F32) for _ in range(2)]
    m1 = [MB.alloc([512], F32) for _ in range(2)]
    m2 = [MB.alloc([512], F32) for _ in range(2)]
    for mt in range(8):
        wi = mt % 2
        load_w(wco_t[wi], w_co_v, mt * 128, 128, ("wco", wi))
        load_w(wgc_t[wi], w_in_v, C_GC + mt * 128, 128, ("wgc", wi))
        load_w(wao_t[wi], w_ao_v, mt * 128, 128, ("wao", wi))
        load_w(wga_t[wi], w_in_v, C_GA + mt * 128, 128, ("wga", wi))
        for j in range(4):
            i = j % 2
            b = i * 4
            to = j * 512
            for ft in range(8):
                mm(psf(b), wco_t[wi][:, ft, :], uT[:, ft, to:to + 512], ft == 0, ft == 7, r=[("wco", wi), "uT"], w=[("ps", b)])
            proj_fm(psf(b + 1), ("ps", b + 1), wgc_t[wi], ("wgc", wi), xT_all, T + to, 512)
            for p_ in range(4):
                mm(psf(b + 2), wao_t[wi][:, p_, :], oT[:, p_, to:to + 512], p_ == 0, p_ == 3, r=[("wao", wi), "oT"], w=[("ps", b + 2)])
            proj_fm(psf(b + 3), ("ps", b + 3), wga_t[wi], ("wga", wi), xT_all, T + to, 512)
            act(sg[i], psf(b + 1), AF.Sigmoid, r=[("ps", b + 1), "gbT"], w=[("sg", i)], bias=gbT[:, 0, mt, :])
            act(sga[i], psf(b + 3), AF.Sigmoid, r=[("ps", b + 3), "gbT"], w=[("sga", i)], bias=gbT[:, 1, mt, :])
            tt("dve", m1[i], sg[i], psf(b), ALU.mult, r=[("sg", i), ("ps", b)], w=[("m1", i)])
            tt("dve", m2[i], sga[i], psf(b + 2), ALU.mult, r=[("sga", i), ("ps", b + 2)], w=[("m2", i)])
            tt("pool", mergedT[:, mt, to:to + 512], m1[i], m2[i], ALU.add, r=[("m1", i), ("m2", i)], w=["mergedT"])
    S.barrier()

    h = A.view(8 * KB, [NT, D], F32)
    wout = A.view(104 * KB, [8, D], BF16)
    load_w(wout[:, :, 0:512], w_out_v, 0, 512, "wout")
    load_w(wout[:, :, 512:1024], w_out_v, 512, 512, "wout")
    for tau in range(NT):
        dma("sp", h[:, tau, :], x_own[tau * 128:(tau + 1) * 128, :], w=[("h", tau)])
        for half in range(2):
            b = (2 * tau + half) % 4
            for c in range(8):
                mm(psf(b), mergedT[:, c, tau * 128:(tau + 1) * 128], wout[:, c, half * 512:(half + 1) * 512],
                   c == 0, c == 7, r=["mergedT", "wout"], w=[("ps", b)])
            tt("dve", h[:, tau, half * 512:(half + 1) * 512], psf(b), h[:, tau, half * 512:(half + 1) * 512], ALU.add,
               r=[("ps", b), ("h", tau)], w=[("h", tau)])
    S.barrier()

    finals = []
    if stage == "mixer":
        for tau in range(NT):
            finals.append(dma("sp", out[tau * 128:(tau + 1) * 128, :], h[:, tau, :], r=[("h", tau)]))
        S.emit("sp", finals)
        return nc, S

    build_peer(nc, S, A, ps, psf, psb, h, locals())
    return nc, S


def build_peer(nc, S, A, ps, psf, psb, h, L):
    mm, tr, act, tt, ts, stt, cp, dma = (L[n] for n in ("mm", "tr", "act", "tt", "ts", "stt", "cp", "dma"))
    cast_load, proj_fm, norm_phase = L["cast_load"], L["proj_fm"], L["norm_phase"]
    WST, wst_n = L["WST"], L["wst_n"]
    ident_f, ident_b, gffnT = L["ident_f"], L["ident_b"], L["gffnT"]
    wqry_v, skeys, pdown, pup, n_fin, out, stage = (L[n] for n in ("wqry_v", "skeys", "pdown", "pup", "n_fin", "out", "stage"))
    NEG = -1.0e30
    NBLK = 128

    dnT_d = nc.dram_tensor("dnT_scr", [NBLK, 128, 8, 128], BF16, kind="Internal").ap()
    up_d = nc.dram_tensor("up_scr", [NBLK * 128, D], BF16, kind="Internal").ap()
    wq_d = nc.dram_tensor("wq_scr", [16, 128, 8, 128], BF16, kind="Internal").ap()

    hnT = A.view(72 * KB, [8, T], BF16)
    PBm = Bump(A, 104 * KB, 190 * KB)
    skT = PBm.alloc([16, 128], BF16)
    lo = PBm.top

    def hnorm_tile(i):
        norm_phase([("sbuf", h[:, i, :], ("h", i))], gffnT, hnT, "hnT", Bump(A, lo, lo + 16 * KB), tile0=i)
    SB_ = Bump(A, lo + 16 * KB, 190 * KB)
    sk_b = [SB_.alloc([128], BF16) for _ in range(2)]
    for j in range(16):
        i = wst_n[0] % 4
        wst_n[0] += 1
        st = WST[i][:, 0, :]
        dma("sp", st, skeys[j, :, :], w=[("wst", i)])
        cp("pool", sk_b[j % 2], st, r=[("wst", i)], w=[("sk_b", j % 2)])
        b = 4 + j % 2
        tr(psb(b)[:, 0:128], sk_b[j % 2], ident_b, r=[("sk_b", j % 2), "ident_b"], w=[("ps", b)])
        act(skT[:, j, :], psb(b)[:, 0:128], AF.Copy, r=[("ps", b)], w=["skT"])

    XB = Bump(A, SB_.top, 190 * KB)
    Dn_b = [XB.alloc([D], BF16) for _ in range(2)]
    Up_b = [XB.alloc([D], BF16) for _ in range(2)]
    DnT_o = [XB.alloc([8, 128], BF16) for _ in range(2)]
    stg = {}

    def pre_load(blk):
        i = wst_n[0] % 4
        wst_n[0] += 1
        i2 = wst_n[0] % 4
        wst_n[0] += 1
        dma("sp", WST[i].rearrange("p a b -> p (a b)"), pdown[blk * 128:(blk + 1) * 128, :], w=[("wst", i)])
        dma("sp", WST[i2].rearrange("p a b -> p (a b)"), pup[blk * 128:(blk + 1) * 128, :], w=[("wst", i2)])
        stg[blk] = (i, i2)

    wq_o = [XB.alloc([8, 128], BF16) for _ in range(2)]
    for j in range(16):
        i = wst_n[0] % 4
        wst_n[0] += 1
        dma("sp", WST[i], wqry_v[:, :, j * 128:(j + 1) * 128], w=[("wst", i)])
        cp("dve", wq_o[j % 2], WST[i], r=[("wst", i)], w=[("wq_o", j % 2)])
        dma("sp", wq_d[j], wq_o[j % 2], r=[("wq_o", j % 2)], w=[("wq_d", j)])
    pre_load(0)
    for blk in range(NBLK):
        if blk + 1 < NBLK:
            pre_load(blk + 1)
        if blk % 8 == 0:
            hnorm_tile(blk // 8)
        i, i2 = stg.pop(blk)
        k2 = blk % 2
        act(Dn_b[k2], WST[i].rearrange("p a b -> p (a b)"), AF.Copy, r=[("wst", i)], w=[("Dn_b", k2)])
        cp("dve", Up_b[k2], WST[i2].rearrange("p a b -> p (a b)"), r=[("wst", i2)], w=[("Up_b", k2)])
        b = 4 + blk % 4
        for c in range(8):
            tr(psb(b)[:, c * 128:(c + 1) * 128], Dn_b[k2][:, c * 128:(c + 1) * 128], ident_b,
               r=[("Dn_b", k2), "ident_b"], w=[("ps", b)])
        cp("dve", DnT_o[k2], psb(b).rearrange("p (c e) -> p c e", c=8), r=[("ps", b)], w=[("DnT_o", k2)])
        dma("sp", dnT_d[blk], DnT_o[k2], r=[("DnT_o", k2)], w=[("dnT_d", blk)])
        dma("sp", up_d[blk * 128:(blk + 1) * 128, :], Up_b[k2], r=[("Up_b", k2)], w=[("up_d", blk)])
    S.barrier()

    GB = Bump(A, lo, 190 * KB)
    s_sb = GB.alloc([2, 16, 128], F32)
    E2 = GB.alloc([2, 8, 128], F32)
    E1q = GB.alloc([2, 8, 128], F32)
    Dg = GB.alloc([2, 8, 128], BF16)
    lo2 = GB.top
    s5 = s_sb.rearrange("p t (h c) k -> p t h c k", c=2)

    for G in range(NT // 2):
        t0 = G * 256
        SU = Bump(A, lo2, 190 * KB)
        qpT = SU.alloc([16, 256], BF16)
        wq_t = [SU.alloc([8, 128], BF16) for _ in range(2)]
        TT = []
        for _t in range(2):
            TT.append(dict(cand=SU.alloc([8, 256], F32), vals=SU.alloc([256], F32),
                           work=[SU.alloc([256], F32) for _ in range(2)], best=SU.alloc([8, 16], F32),
                           m3=SU.alloc([8, 8], F32), thr=SU.alloc([8, 1], F32), dd=SU.alloc([8, 16], F32),
                           Z=SU.alloc([8, 1], F32), rZ=SU.alloc([8, 1], F32), gthr=SU.alloc([8, 1], F32)))
        for j in range(16):
            wi = j % 2
            dma("sp", wq_t[wi], wq_d[j], r=[("wq_d", j)], w=[("wqp", wi)])
            b = 4 + j % 2
            proj_fm(psf(b, 0, 256), ("ps", b), wq_t[wi], ("wqp", wi), hnT, t0, 256)
            act(qpT[:, j, :], psf(b, 0, 256), AF.Copy, r=[("ps", b)], w=["qpT"])
        def tile_gen(tt_):
            cand, vals, work, best, m3, thr, dd, Z, rZ, gthr = (TT[tt_][n] for n in
                ("cand", "vals", "work", "best", "m3", "thr", "dd", "Z", "rZ", "gthr"))
            K_ = lambda n: (n, tt_)
            for jj in range(4):
                b = 6 + jj % 2
                for j4 in range(4):
                    j = jj * 4 + j4
                    mm(psf(b, j4 * 128, 128), qpT[:, j, tt_ * 128:(tt_ + 1) * 128], skT[:, j, :], True, True,
                       r=["qpT", "skT"], w=[("ps", b)])
                cp("dve", s_sb[:, tt_, jj * 4:(jj + 1) * 4, :], psf(b).rearrange("p (j k) -> p j k", j=4),
                   r=[("ps", b)], w=[("s_sb", tt_, jj)])
                yield
            skeys_ = [("s_sb", tt_, jj) for jj in range(4)]
            v_a = vals.rearrange("p (h c a o) -> p h c a o", h=8, c=2, a=16, o=1)
            v_b = vals.rearrange("p (h c o a) -> p h c o a", h=8, c=2, a=16, o=1)
            v3 = vals.rearrange("p (j a) -> p j a", j=16)
            for j in range(16):
                src = s_sb[:, tt_, j, :]
                sk = [("s_sb", tt_, j // 4)]
                S.op("dve", lambda e, j=j, src=src: e.max(out=v3[:, j, 0:8], in_=src), r=sk, w=[K_("vals")])
                yield
                S.op("dve", lambda e, j=j, src=src: e.match_replace(out=work[0][:, 0:128], in_to_replace=v3[:, j, 0:8],
                                                                    in_values=src, imm_value=NEG),
                     r=sk + [K_("vals")], w=[K_("work0")])
                yield
                S.op("dve", lambda e, j=j: e.max(out=v3[:, j, 8:16], in_=work[0][:, 0:128]), r=[K_("work0")], w=[K_("vals")])
                yield
            for hd in range(8):
                tt("dve", cand[:, hd, :].rearrange("p (a b) -> p a b", a=16),
                   v_a[:, hd, 0, :, 0:1].to_broadcast([128, 16, 16]),
                   v_b[:, hd, 1, 0:1, :].to_broadcast([128, 16, 16]), ALU.add, r=[K_("vals")], w=[K_("cand")])
                yield
            for hd in range(8):
                ch = cand[:, hd, :]
                S.op("dve", lambda e, hd=hd, ch=ch: e.max(out=best[:, hd, 0:8], in_=ch), r=[K_("cand")], w=[K_("best")])
                yield
                S.op("dve", lambda e, hd=hd, ch=ch: e.match_replace(out=work[0], in_to_replace=best[:, hd, 0:8],
                                                                    in_values=ch, imm_value=NEG),
                     r=[K_("cand"), K_("best")], w=[K_("work0")])
                yield
                S.op("dve", lambda e, hd=hd: e.max(out=best[:, hd, 8:16], in_=work[0]), r=[K_("work0")], w=[K_("best")])
                yield
                S.op("dve", lambda e, hd=hd: e.match_replace(out=work[1], in_to_replace=best[:, hd, 8:16],
                                                             in_values=work[0], imm_value=NEG),
                     r=[K_("work0"), K_("best")], w=[K_("work1")])
                yield
                S.op("dve", lambda e, hd=hd: e.max(out=m3[:, hd, :], in_=work[1]), r=[K_("work1")], w=[K_("m3")])
                yield
            tt("dve", thr, best[:, :, 15:16], m3[:, :, 0:1], ALU.add, r=[K_("best"), K_("m3")], w=[K_("thr")])
            yield
            ts("dve", thr, thr, 0.5, None, ALU.mult, r=[K_("thr")], w=[K_("thr")])
            yield
            tt("dve", dd, best, best[:, :, 0:1].to_broadcast([128, 8, 16]), ALU.subtract, r=[K_("best")], w=[K_("dd")])
            yield
            act(dd, dd, AF.Exp, r=[K_("dd")], w=[K_("dd")])
            S.op("dve", lambda e: e.tensor_reduce(out=Z, in_=dd, axis=AX.X, op=ALU.add), r=[K_("dd")], w=[K_("Z")])
            yield
            S.op("dve", lambda e: e.reciprocal(out=rZ, in_=Z), r=[K_("Z")], w=[K_("rZ")])
            yield
            s1v = s5[:, tt_, :, 0, :]
            s2v = s5[:, tt_, :, 1, :]
            tt("dve", gthr, thr, best[:, :, 0:1], ALU.subtract, r=[K_("thr"), K_("best")], w=[K_("gthr")])
            yield
            act(gthr, gthr, AF.Exp, r=[K_("gthr")], w=[K_("gthr")])
            tt("dve", gthr, gthr, rZ, ALU.mult, r=[K_("gthr"), K_("rZ")], w=[K_("gthr")])
            yield
            tt("dve", E1q[:, tt_, :, :], s1v, thr[:, :, 0:1].to_broadcast([128, 8, 128]), ALU.subtract,
               r=skeys_ + [K_("thr")], w=[K_("E1q")])
            yield
            act(E1q[:, tt_, :, :], E1q[:, tt_, :, :], AF.Exp, r=[K_("E1q")], w=[K_("E1q")])
            act(E2[:, tt_, :, :], s2v, AF.Exp, r=skeys_, w=[K_("E2")])
            for hd in range(8):
                ts("dve", Dg[:, tt_, hd, :], ident_f, gthr[:, hd, :], None, ALU.mult, r=["ident_f", K_("gthr")], w=[("Dg", tt_, hd)])
                yield

        gens = [tile_gen(0), tile_gen(1)]
        alive = [True, True]
        while any(alive):
            for gi in range(2):
                if alive[gi]:
                    try:
                        next(gens[gi])
                    except StopIteration:
                        alive[gi] = False
        S.barrier()

        LB = Bump(A, lo2, 190 * KB)
        DnT = [LB.alloc([8, 128], BF16) for _ in range(3)]
        Upb = [LB.alloc([D], BF16) for _ in range(4)]
        GA = [LB.alloc([256], BF16) for _ in range(3)]
        mt_ = [LB.alloc([2, 8, 128], F32) for _ in range(2)]
        mb = [LB.alloc([2, 8, 128], BF16) for _ in range(2)]
        WA = [LB.alloc([256], BF16) for _ in range(2)]
        E2f = E2.rearrange("p t h k -> p (t h) k")
        E1qf = E1q.rearrange("p t h k -> p (t h) k")

        def loads(i):
            dma("sp", DnT[i % 3], dnT_d[i], r=[("dnT_d", i)], w=[("DnT", i % 3)])
            dma("sp", Upb[i % 4], up_d[i * 128:(i + 1) * 128, :], r=[("up_d", i)], w=[("Upb", i % 4)])

        loads(0)
        N = NBLK if CUT[0] != 77 else 2
        for it in range(N + 2):
            if it + 1 < N:
                loads(it + 1)
            if it < N:
                i = it
                ND = 9
                mt_f = mt_[i % 2].rearrange("p t h k -> p (t h) k")
                tt("dve", mt_f[:, 0:ND, :], E2f[:, 0:ND, :], E1qf[:, 0:ND, i:i + 1].to_broadcast([128, ND, 128]), ALU.mult,
                   r=["E2", "E1q"], w=[("mt", i % 2)])
                for ch in range(ND, 16):
                    act(mt_f[:, ch, :], E2f[:, ch, :], AF.Copy, r=["E2", "E1q"], w=[("mt1", i % 2, ch)],
                        scale=E1qf[:, ch, i:i + 1])
                b = 4 + i % 2
                for c in range(8):
                    mm(psf(b, 0, 256), DnT[i % 3][:, c, :], hnT[:, c, t0:t0 + 256], c == 0, c == 7,
                       r=[("DnT", i % 3)], w=[("ps", b)])
                act(GA[i % 3], psf(b, 0, 256), AF.Gelu, r=[("ps", b)], w=[("GA", i % 3)])
            if 1 <= it <= N:
                i = it - 1
                stt(mb[i % 2], mt_[i % 2], 1.0, mt_[i % 2], ALU.is_ge, ALU.mult, r=[("mt", i % 2)] + [("mt1", i % 2, ch) for ch in range(9, 16)], w=[("mb", i % 2)])
                b = 6 + i % 2
                for tt_ in range(2):
                    for hd in (range(8) if CUT[0] != 78 else [0, 7]):
                        mm(psf(b, tt_ * 128, 128), mb[i % 2][:, tt_, hd, :], Dg[:, tt_, hd, :], hd == 0, hd == 7,
                           r=[("mb", i % 2), "Dg"], w=[("ps", b)])
            if 2 <= it <= N + 1:
                i = it - 2
                b = 6 + i % 2
                tt("dve", WA[i % 2], psf(b, 0, 256), GA[i % 3], ALU.mult, r=[("ps", b), ("GA", i % 3)], w=[("WA", i % 2)])
                for tt_ in range(2):
                    for half in range(2):
                        bo = tt_ * 2 + half
                        mm(psf(bo), WA[i % 2][:, tt_ * 128:(tt_ + 1) * 128], Upb[i % 4][:, half * 512:(half + 1) * 512],
                           i == 0, i == N - 1, r=[("WA", i % 2), ("Upb", i % 4)], w=[("ps", bo)])
        for tt_ in range(2):
            for half in range(2):
                bo = tt_ * 2 + half
                tau = 2 * G + tt_
                tt("dve", h[:, tau, half * 512:(half + 1) * 512], psf(bo), h[:, tau, half * 512:(half + 1) * 512], ALU.add,
                   r=[("ps", bo), ("h", tau)], w=[("h", tau)])
        S.barrier()

    finals = []
    if stage == "full_h2":
        for tau in range(NT):
            finals.append(dma("sp", out[tau * 128:(tau + 1) * 128, :], h[:, tau, :], r=[("h", tau)]))
        S.emit("sp", finals)
        return

    FB = Bump(A, 72 * KB, 190 * KB)
    fin_bc = FB.alloc([D], F32)
    junk = FB.alloc([D], BF16)
    ob = [FB.alloc([D], F32) for _ in range(2)]
    ss = [FB.alloc([1], F32) for _ in range(2)]
    sd = [FB.alloc([1], F32) for _ in range(2)]
    rstd = [FB.alloc([1], F32) for _ in range(2)]
    dma("sp", fin_bc, n_fin.partition_broadcast(128), w=["fin_bc"])
    for tau in range(NT):
        k = tau % 2
        act(junk, h[:, tau, :], AF.Square, r=[("h", tau)], w=["fjunk", ("fss", k)], accum_out=ss[k])
        act(sd[k], ss[k], AF.Sqrt, r=[("fss", k)], w=[("fsd", k)], scale=1.0 / D, bias=1e-6)
        S.op("dve", lambda e, k=k: e.reciprocal(out=rstd[k], in_=sd[k]), r=[("fsd", k)], w=[("frstd", k)])
        stt(ob[k], h[:, tau, :], rstd[k], fin_bc, ALU.mult, ALU.mult, r=[("h", tau), ("frstd", k), "fin_bc"], w=[("ob", k)])
        finals.append(dma("sp", out[tau * 128:(tau + 1) * 128, :], ob[k], r=[("ob", k)]))
    S.emit("sp", finals)


def make_consts():
    c = {}
    c["c_ident"] = np.eye(128, dtype=np.float32)
    k = np.arange(128)[:, None]
    q = np.arange(128)[None, :]
    c["c_mask"] = np.concatenate([(k >= q), (k <= q)], axis=1).astype(np.float32)
    e1 = np.zeros((128, 516), np.float32)
    e1[np.arange(128), 4 + 4 * np.arange(128)] = 1.0
    e2 = np.zeros((128, 2064), np.float32)
    e2[np.arange(128), 16 + 16 * np.arange(128)] = 1.0
    c["c_e1"], c["c_e2"] = e1, e2
    inv_freq = (np.float32(500000.0) ** (-np.arange(8, dtype=np.float32) * np.float32(2.0 / 16))).astype(np.float32)
    rope = np.zeros((128, 2), np.float32)
    for p in range(128):
        d = p % 64
        if d < 16:
            rope[p, 0] = inv_freq[d % 8]
            rope[p, 1] = -1.0 if d < 8 else 1.0
    c["c_rope"] = rope
    return c


def make_in_maps(inputs, stage="full"):
    x = np.asarray(inputs["x"], np.float32)
    positions = np.asarray(inputs["positions"], np.int32)
    consts = make_consts()
    shared = {
        "w_in": np.ascontiguousarray(inputs["w_in"][0]),
        "conv_w": np.ascontiguousarray(inputs["conv_w"][0]),
        "w_conv_out": np.ascontiguousarray(inputs["w_conv_out"][0]),
        "w_attn_out": np.ascontiguousarray(inputs["w_attn_out"][0]),
        "gate_bias": np.ascontiguousarray(inputs["gate_bias"][0]),
        "w_out": np.ascontiguousarray(inputs["w_out"][0]),
        "norm_mix": np.ascontiguousarray(inputs["norm_mix"][0]),
        "norm_ffn": np.ascontiguousarray(inputs["norm_ffn"][0]),
        "final_norm": np.ascontiguousarray(inputs["final_norm"]),
        "peer_w_query": np.ascontiguousarray(inputs["peer_w_query"][0]),
        "peer_sub_keys": np.ascontiguousarray(inputs["peer_sub_keys"][0]).reshape(16, 128, 128),
        "peer_down": np.ascontiguousarray(inputs["peer_down"][0]),
        "peer_up": np.ascontiguousarray(inputs["peer_up"][0]),
    }
    shared = {k: np.asarray(v, np.float32) for k, v in shared.items()}
    shared.update(consts)
    if stage not in ("full", "full_h2"):
        shared.pop("peer_down"); shared.pop("peer_up")
    maps = []
    for core in range(8):
        b, q = core // 4, core % 4
        m = dict(shared)
        m["x_own"] = np.ascontiguousarray(x[b, q * T:(q + 1) * T])
        if q > 0:
            m["x_halo"] = np.ascontiguousarray(x[b, (q - 1) * T:q * T])
            ph = positions[b, (q - 1) * T:q * T]
        else:
            m["x_halo"] = np.zeros((T, D), np.float32)
            ph = np.zeros((T,), np.int32)
        m["pos"] = np.ascontiguousarray(np.concatenate([ph, positions[b, q * T:(q + 1) * T]])[None, :]).astype(np.int32)
        m["halo_flag"] = np.full((128, 1), 1.0 if q > 0 else 0.0, np.float32)
        maps.append(m)
    return maps


_CACHE = {}


def kernel(**inputs):
    stage = inputs.pop("_stage", "full")
    if stage not in _CACHE:
        _CACHE[stage] = build(stage)
    nc, _ = _CACHE[stage]
    maps = make_in_maps(inputs, stage)
    res = run_bass_kernel_spmd(nc, maps, core_ids=list(range(8)))
    outs = [np.asarray(r["out"], np.float32) for r in res.results]
    full = np.stack([np.concatenate(outs[0:4], axis=0), np.concatenate(outs[4:8], axis=0)], axis=0)
    return full.astype(np.float32)
```
